# Optimizing a Trainium2 kernel written in Bass

```python
import math
import jax, jax.numpy as jnp
from jax import lax
import numpy as np

D_MODEL = 1024
BATCH = 4
SEQ = 8192
DEPTH = 1

GRID_W = 64
CTX_LEN = 256
DA_HEADS = 4
DA_QK = 64
DA_V = 2 * DA_QK
ML_HEADS = 4
ML_QK = 64
ML_V = 128
D_MIX = DA_HEADS * DA_V + ML_HEADS * ML_V
D_FF = 4 * D_MODEL
ROPE_BASE = 10000.0
Q_BLOCK = 128
ML_CHUNK = 64
EPS = 1e-6

DA_Q_COLS = DA_HEADS * 2 * DA_QK
DA_K_COLS = DA_HEADS * 2 * DA_QK
DA_V_COLS = DA_HEADS * DA_V
ML_Q_COLS = ML_HEADS * ML_QK
ML_K_COLS = ML_HEADS * ML_QK
ML_V_COLS = ML_HEADS * ML_V
ML_O_COLS = ML_HEADS * ML_V
ML_G_COLS = 4 * ML_HEADS
IN_SPLITS = (DA_Q_COLS,
             DA_Q_COLS + DA_K_COLS,
             DA_Q_COLS + DA_K_COLS + DA_V_COLS,
             DA_Q_COLS + DA_K_COLS + DA_V_COLS + ML_Q_COLS,
             DA_Q_COLS + DA_K_COLS + DA_V_COLS + ML_Q_COLS + ML_K_COLS,
             DA_Q_COLS + DA_K_COLS + DA_V_COLS + ML_Q_COLS + ML_K_COLS + ML_V_COLS,
             DA_Q_COLS + DA_K_COLS + DA_V_COLS + ML_Q_COLS + ML_K_COLS + ML_V_COLS + ML_O_COLS)
IN_COLS = IN_SPLITS[-1] + ML_G_COLS

kernel_name = "hybrid_diffattn_mlstm_dit_block"


def rmsnorm(x, g):
    xf = x.astype(jnp.float32)
    y = xf * lax.rsqrt(jnp.mean(xf * xf, axis=-1, keepdims=True) + EPS)
    return (y * g.astype(jnp.float32)).astype(x.dtype)


def axial_rope(T):
    rows = T // GRID_W
    row = jnp.repeat(jnp.arange(rows, dtype=jnp.float32), GRID_W)
    col = jnp.tile(jnp.arange(GRID_W, dtype=jnp.float32), rows)
    half = DA_QK // 2
    inv = ROPE_BASE ** (-jnp.arange(0, half, 2, dtype=jnp.float32) / half)
    ar = row[:, None] * inv
    ac = col[:, None] * inv
    ang = jnp.concatenate([ar, ar, ac, ac], axis=-1)
    return jnp.cos(ang), jnp.sin(ang)


def apply_rope(x, cos, sin):
    def rot(u):
        u1, u2 = jnp.split(u, 2, axis=-1)
        return jnp.concatenate([-u2, u1], axis=-1)
    xr, xc = jnp.split(x, 2, axis=-1)
    xrot = jnp.concatenate([rot(xr), rot(xc)], axis=-1)
    return (x * cos + xrot * sin).astype(x.dtype)


def project(xm, w_in_l, b_gate_l):
    B_, T_ = xm.shape[0], xm.shape[1]
    h = xm @ w_in_l
    dq, dk, dv, mq, mk, mv, mo, mg = jnp.split(h, IN_SPLITS, axis=-1)
    dq = dq.reshape(B_, T_, DA_HEADS, 2, DA_QK).transpose(0, 2, 3, 1, 4)
    dk = dk.reshape(B_, T_, DA_HEADS, 2, DA_QK).transpose(0, 2, 3, 1, 4)
    dv = dv.reshape(B_, T_, DA_HEADS, DA_V).transpose(0, 2, 1, 3)
    mq = mq.reshape(B_, T_, ML_HEADS, ML_QK).transpose(0, 2, 1, 3)
    mk = mk.reshape(B_, T_, ML_HEADS, ML_QK).transpose(0, 2, 1, 3) * (ML_QK ** -0.5)
    mv = mv.reshape(B_, T_, ML_HEADS, ML_V).transpose(0, 2, 1, 3)
    mo = jax.nn.sigmoid(mo)
    mg = (mg.reshape(B_, T_, 4, ML_HEADS) + b_gate_l.reshape(4, ML_HEADS)).transpose(2, 0, 3, 1)
    return dq, dk, dv, mq, mk, mv, mo, mg


def diff_attn_core(q, k, v, lam):
    s = jnp.einsum('bhmqd,bhmkd->bhmqk', q, k).astype(jnp.float32) * (DA_QK ** -0.5)
    p = jax.nn.softmax(s, axis=-1)
    a = p[:, :, 0] - lam * p[:, :, 1]
    return jnp.einsum('bhqk,bhkv->bhqv', a.astype(v.dtype), v)


def diff_attn_latent(q, k_all, v_all, lam):
    B_, H_, _, T_, d = q.shape
    nb = T_ // Q_BLOCK
    qb = jnp.moveaxis(q.reshape(B_, H_, 2, nb, Q_BLOCK, d), 3, 0)
    out = lax.map(lambda qblk: diff_attn_core(qblk, k_all, v_all, lam), qb)
    return jnp.moveaxis(out, 0, 2).reshape(B_, H_, T_, v_all.shape[-1])


def mlstm_chunked(q, k, v, li, lf, state):
    out_dtype = v.dtype
    q, k, v = q.astype(jnp.float32), k.astype(jnp.float32), v.astype(jnp.float32)
    B_, H_, T_, dk = q.shape
    dv = v.shape[-1]
    nc = T_ // ML_CHUNK

    def chunks(a):
        a = a.reshape((B_, H_, nc, ML_CHUNK) + a.shape[3:])
        return jnp.moveaxis(a, 2, 0)

    tril = jnp.tril(jnp.ones((ML_CHUNK, ML_CHUNK), dtype=bool))

    def step(carry, inp):
        C, n, m = carry
        qc, kc, vc, lic, lfc = inp
        b = jnp.cumsum(lfc, axis=-1)
        dmat = b[..., :, None] - b[..., None, :] + lic[..., None, :]
        dmat = jnp.where(tril, dmat, -jnp.inf)
        inter = b + m[..., None]
        m_t = jnp.maximum(inter, jnp.max(dmat, axis=-1))
        s = jnp.einsum('bhtd,bhsd->bhts', qc, kc) * jnp.exp(dmat - m_t[..., None])
        w_inter = jnp.exp(inter - m_t)
        num = jnp.einsum('bhts,bhsv->bhtv', s, vc) + w_inter[..., None] * jnp.einsum('bhvd,bhtd->bhtv', C, qc)
        den = jnp.sum(s, axis=-1) + w_inter * jnp.einsum('bhd,bhtd->bht', n, qc)
        h = num / jnp.maximum(jnp.abs(den), jnp.exp(-m_t))[..., None]
        b_last = b[..., -1]
        g = b_last[..., None] - b + lic
        m_new = jnp.maximum(b_last + m, jnp.max(g, axis=-1))
        w = jnp.exp(g - m_new[..., None])
        decay = jnp.exp(b_last + m - m_new)
        C = decay[..., None, None] * C + jnp.einsum('bhsv,bhsd->bhvd', vc * w[..., None], kc)
        n = decay[..., None] * n + jnp.einsum('bhs,bhsd->bhd', w, kc)
        return (C, n, m_new), h

    state, h = lax.scan(step, state, (chunks(q), chunks(k), chunks(v), chunks(li), chunks(lf)))
    h = jnp.moveaxis(h, 0, 2).reshape(B_, H_, T_, dv)
    return h.astype(out_dtype), state


def mlstm_zero_state(B_):
    return (jnp.zeros((B_, ML_HEADS, ML_V, ML_QK), jnp.float32),
            jnp.zeros((B_, ML_HEADS, ML_QK), jnp.float32),
            jnp.zeros((B_, ML_HEADS), jnp.float32))


def gates(mg):
    mg = mg.astype(jnp.float32)
    return mg[0], jax.nn.log_sigmoid(mg[1]), mg[2], jax.nn.log_sigmoid(mg[3])


def merge_heads(da_h, ml_h, mo, subln_g_l, ml_norm_g_l, lambda_init, w_out_l):
    B_, _, T_, _ = da_h.shape
    da = rmsnorm(da_h, subln_g_l) * (1.0 - lambda_init)
    da = da.transpose(0, 2, 1, 3).reshape(B_, T_, DA_HEADS * DA_V)
    ml = rmsnorm(ml_h, ml_norm_g_l.reshape(ML_HEADS, 1, ML_V))
    ml = ml.transpose(0, 2, 1, 3).reshape(B_, T_, ML_HEADS * ML_V) * mo
    return jnp.concatenate([da, ml], axis=-1) @ w_out_l


def sq_relu_mlp(x, w1, w2):
    return jnp.square(jax.nn.relu(x @ w1)) @ w2


def hybrid_mixer(xm, cm, w_in_l, b_gate_l, lq1, lk1, lq2, lk2, subln_g_l, ml_norm_g_l, w_out_l,
                 cos, sin, lambda_init, update_ctx):
    B_ = xm.shape[0]
    dq, dk, dv, mq, mk, mv, mo, mg = project(xm, w_in_l, b_gate_l)
    cdq, cdk, cdv, cmq, cmk, cmv, cmo, cmg = project(cm, w_in_l, b_gate_l)
    lam = (jnp.exp(jnp.sum(lq1.astype(jnp.float32) * lk1.astype(jnp.float32)))
           - jnp.exp(jnp.sum(lq2.astype(jnp.float32) * lk2.astype(jnp.float32))) + lambda_init)

    q_lat = apply_rope(dq, cos, sin)
    k_all = jnp.concatenate([cdk, apply_rope(dk, cos, sin)], axis=3)
    v_all = jnp.concatenate([cdv, dv], axis=2)
    da_lat = diff_attn_latent(q_lat, k_all, v_all, lam)

    li_f, lf_f, li_b, lf_b = gates(mg)
    cli_f, clf_f, cli_b, clf_b = gates(cmg)
    flip = lambda a: jnp.flip(a, axis=2)
    zero = mlstm_zero_state(B_)
    hc_f, st_f = mlstm_chunked(cmq, cmk, cmv, cli_f, clf_f, zero)
    hc_b_rev, st_b = mlstm_chunked(flip(cmq), flip(cmk), flip(cmv), flip(cli_b), flip(clf_b), zero)
    h_f, _ = mlstm_chunked(mq, mk, mv, li_f, lf_f, st_f)
    h_b_rev, _ = mlstm_chunked(flip(mq), flip(mk), flip(mv), flip(li_b), flip(lf_b), st_b)
    ml_lat = h_f + flip(h_b_rev)

    y_lat = merge_heads(da_lat, ml_lat, mo, subln_g_l, ml_norm_g_l, lambda_init, w_out_l)
    if update_ctx:
        da_ctx = diff_attn_core(cdq, cdk, cdv, lam)
        ml_ctx = hc_f + flip(hc_b_rev)
        y_ctx = merge_heads(da_ctx, ml_ctx, cmo, subln_g_l, ml_norm_g_l, lambda_init, w_out_l)
        return y_lat, y_ctx
    return y_lat, None


def setup_inputs(seed: int = 0) -> dict:
    key = jax.random.key(seed)
    ks = jax.random.split(key, 20)
    f32 = jnp.float32
    nrm = lambda k, shape, s: jax.random.normal(k, shape, f32) * s
    f_bias = jnp.tile(jnp.linspace(3.0, 6.0, ML_HEADS, dtype=f32), 2)
    gate_offset = jnp.concatenate([jnp.zeros((ML_HEADS,), f32), f_bias[:ML_HEADS],
                                   jnp.zeros((ML_HEADS,), f32), f_bias[ML_HEADS:]])
    return {
        "x": nrm(ks[0], (BATCH, SEQ, D_MODEL), 1.0),
        "c": nrm(ks[1], (BATCH, D_MODEL), 1.0),
        "ctx": nrm(ks[2], (BATCH, CTX_LEN, D_MODEL), 1.0),
        "c_ctx": nrm(ks[3], (D_MODEL,), 1.0),
        "w_ada": nrm(ks[4], (DEPTH, D_MODEL, 6 * D_MODEL), D_MODEL ** -0.5),
        "b_ada": nrm(ks[5], (DEPTH, 6 * D_MODEL), 0.02),
        "norm1_g": 1.0 + nrm(ks[6], (DEPTH, D_MODEL), 0.02),
        "norm2_g": 1.0 + nrm(ks[7], (DEPTH, D_MODEL), 0.02),
        "w_in": nrm(ks[8], (DEPTH, D_MODEL, IN_COLS), D_MODEL ** -0.5),
        "b_gate": gate_offset + nrm(ks[9], (DEPTH, ML_G_COLS), 0.1),
        "lam_q1": nrm(ks[10], (DEPTH, DA_QK), 0.1),
        "lam_k1": nrm(ks[11], (DEPTH, DA_QK), 0.1),
        "lam_q2": nrm(ks[12], (DEPTH, DA_QK), 0.1),
        "lam_k2": nrm(ks[13], (DEPTH, DA_QK), 0.1),
        "subln_g": 1.0 + nrm(ks[14], (DEPTH, DA_V), 0.02),
        "mlstm_norm_g": 1.0 + nrm(ks[15], (DEPTH, ML_HEADS * ML_V), 0.02),
        "w_out": nrm(ks[16], (DEPTH, D_MIX, D_MODEL), D_MIX ** -0.5),
        "w_fc1": nrm(ks[17], (DEPTH, D_MODEL, D_FF), D_MODEL ** -0.5),
        "w_fc2": nrm(ks[18], (DEPTH, D_FF, D_MODEL), D_FF ** -0.5),
        "final_g": 1.0 + nrm(ks[19], (D_MODEL,), 0.02),
    }


def reference(x, c, ctx, c_ctx, w_ada, b_ada, norm1_g, norm2_g, w_in, b_gate, lam_q1, lam_k1, lam_q2, lam_k2,
              subln_g, mlstm_norm_g, w_out, w_fc1, w_fc2, final_g):
    T = x.shape[1]
    cos, sin = axial_rope(T)
    for l in range(DEPTH):
        update_ctx = l < DEPTH - 1
        lambda_init = 0.8 - 0.6 * math.exp(-0.3 * l)
        mod = jax.nn.silu(c) @ w_ada[l] + b_ada[l]
        mod_c = jax.nn.silu(c_ctx) @ w_ada[l] + b_ada[l]
        sh1, sc1, g1, sh2, sc2, g2 = jnp.split(mod[:, None, :], 6, axis=-1)
        csh1, csc1, cg1, csh2, csc2, cg2 = jnp.split(mod_c[None, None, :], 6, axis=-1)
        xm = rmsnorm(x, norm1_g[l]) * (1.0 + sc1) + sh1
        cm = rmsnorm(ctx, norm1_g[l]) * (1.0 + csc1) + csh1
        y, y_ctx = hybrid_mixer(xm, cm, w_in[l], b_gate[l], lam_q1[l], lam_k1[l], lam_q2[l], lam_k2[l],
                                subln_g[l], mlstm_norm_g[l], w_out[l], cos, sin, lambda_init, update_ctx)
        x = x + g1 * y
        x = x + g2 * sq_relu_mlp(rmsnorm(x, norm2_g[l]) * (1.0 + sc2) + sh2, w_fc1[l], w_fc2[l])
        if update_ctx:
            ctx = ctx + cg1 * y_ctx
            ctx = ctx + cg2 * sq_relu_mlp(rmsnorm(ctx, norm2_g[l]) * (1.0 + csc2) + csh2, w_fc1[l], w_fc2[l])
    return rmsnorm(x, final_g)
```

```python
import numpy as np
import ml_dtypes
import concourse.bass as bass
import concourse.mybir as mybir
from concourse.bass_utils import run_bass_kernel_spmd
from contextlib import ExitStack

F32 = mybir.dt.float32
BF16 = mybir.dt.bfloat16
AF = mybir.ActivationFunctionType
ALU = mybir.AluOpType
AX = mybir.AxisListType


class Tok:
    def __init__(self, name="tok"):
        self.name = name
        self.writer = None
        self.readers = []


class TT(Tok):
    def __init__(self, t, name):
        Tok.__init__(self, name)
        self.t = t


class Sched:
    ENG = ("pe", "act", "dve", "pool", "sp")
    LIMIT = 20000

    def __init__(self, nc, es, ndma=24):
        self.nc = nc
        self.es = es
        self.mes = es
        self.ops = {e: [] for e in self.ENG}
        self.cnt = {e: 0 for e in self.ENG}
        self.epoch = {e: 0 for e in self.ENG}
        self.seen = {e: {} for e in self.ENG}
        self.sems = {}
        self.pend = {e: ([], []) for e in self.ENG}
        self.ndma = ndma
        self.dma_issued = [0] * ndma
        self.dma_rr = 0
        self.out_tokens = []
        self.nblk = 0
        self.uid = 0
        for i in range(ndma):
            self._sem(("dma", i))
        for e in self.ENG:
            self._sem((e, 0))

    def _sem(self, key):
        if key not in self.sems:
            nm = "s_" + "_".join(str(k) for k in key)
            self.sems[key] = self.es.enter_context(self.nc.semaphore(nm))
        return self.sems[key]

    def sb(self, name, shape, dtype):
        self.uid += 1
        t = self.mes.enter_context(self.nc.sbuf_tensor(f"{name}_{self.uid}", list(shape), dtype))
        return TT(t, name)

    def ps(self, name, shape, dtype):
        self.uid += 1
        t = self.mes.enter_context(self.nc.psum_tensor(f"{name}_{self.uid}", list(shape), dtype))
        return TT(t, name)

    def _wait(self, eng, tok):
        if tok is None:
            return
        key, val = tok
        if self.seen[eng].get(key, 0) >= val:
            return
        if eng == "pe" and key[0] == "pe":
            return
        self.seen[eng][key] = val
        sem = self.sems[key]
        self.ops[eng].append(lambda e, sem=sem, val=val: e.wait_ge(sem, val))

    def _deps(self, eng, reads, writes):
        for b in reads:
            self._wait(eng, b.writer)
        for b in writes:
            self._wait(eng, b.writer)
            for r in b.readers:
                self._wait(eng, r)

    def op(self, eng, fn, reads=(), writes=(), inc=True):
        self._deps(eng, reads, writes)
        pr, pw = self.pend[eng]
        pr.extend(reads)
        pw.extend(writes)
        if not inc:
            self.ops[eng].append(lambda e, fn=fn: fn(e))
            return None
        if self.cnt[eng] >= self.LIMIT:
            self.epoch[eng] += 1
            self.cnt[eng] = 0
            self._sem((eng, self.epoch[eng]))
        key = (eng, self.epoch[eng])
        sem = self.sems[key]
        self.cnt[eng] += 1
        tok = (key, self.cnt[eng])
        self.ops[eng].append(lambda e, fn=fn, sem=sem: fn(e).then_inc(sem, 1))
        for b in pw:
            b.writer = tok
            b.readers = []
        for b in pr:
            if b.writer is not tok:
                b.readers.append(tok)
        self.pend[eng] = ([], [])
        return tok

    def dma(self, eng, out_ap, in_ap, reads=(), writes=(), out=False, **kw):
        self._deps(eng, reads, writes)
        i = self.dma_rr
        self.dma_rr = (self.dma_rr + 1) % self.ndma
        key = ("dma", i)
        if self.dma_issued[i] > 0:
            self._wait(eng, (key, 16 * self.dma_issued[i]))
        sem = self.sems[key]
        self.dma_issued[i] += 1
        tok = (key, 16 * self.dma_issued[i])
        self.ops[eng].append(
            lambda e, sem=sem, o=out_ap, a=in_ap, kw=kw: e.dma_start(out=o, in_=a, **kw).then_inc(sem, 16))
        for b in writes:
            b.writer = tok
            b.readers = []
        for b in reads:
            b.readers.append(tok)
        if out:
            self.out_tokens.append(tok)
        return tok

    def flush(self, final=False):
        if final:
            for tok in self.out_tokens:
                self._wait("sp", tok)
            for e in ("pe", "act", "dve", "pool"):
                if self.cnt[e] > 0:
                    self._wait("sp", ((e, self.epoch[e]), self.cnt[e]))
        else:
            self.barrier()
        ops = self.ops
        self.ops = {e: [] for e in self.ENG}
        self.nblk += 1
        with self.nc.Block() as block:
            @block.tensor
            def _(pe):
                for f in ops["pe"]:
                    f(pe)

            @block.scalar
            def _(act):
                for f in ops["act"]:
                    f(act)

            @block.vector
            def _(dve):
                for f in ops["dve"]:
                    f(dve)

            @block.gpsimd
            def _(pool):
                for f in ops["pool"]:
                    f(pool)

            @block.sync
            def _(sp):
                for f in ops["sp"]:
                    f(sp)

    def barrier(self):
        for e in ("pe", "act", "dve", "pool"):
            if self.cnt[e] > 0:
                self._wait("sp", ((e, self.epoch[e]), self.cnt[e]))
        for i in range(self.ndma):
            if self.dma_issued[i] > 0:
                self._wait("sp", (("dma", i), 16 * self.dma_issued[i]))
        if self.cnt["sp"] >= self.LIMIT:
            self.epoch["sp"] += 1
            self.cnt["sp"] = 0
            self._sem(("sp", self.epoch["sp"]))
        key = ("sp", self.epoch["sp"])
        sem = self.sems[key]
        self.cnt["sp"] += 1
        tok = (key, self.cnt["sp"])
        self.ops["sp"].append(lambda e, sem=sem: e.sem_inc(sem, 1))
        for e in ("pe", "act", "dve", "pool"):
            self._wait(e, tok)

    def finish(self):
        self.flush(final=True)


D = 1024
T = 8192
TOWN = 4096
NCTX = 256
NKEY = T + NCTX
NT = NKEY // 128
DFF = 4096
EPS = 1e-6
NFM = 20
TM0 = NFM * 128
WCOLS = TM0 + 1808
C_DV, C_MV, C_MO, C_MK, C_G = TM0, TM0 + 512, TM0 + 1024, TM0 + 1536, TM0 + 1792
SM_CT = 0
SM_BADA = 16
SM_NG = 64
SM_LAM = 80
SM_SUB = 336
SM_MLG = 337
SM_BG = 341
SM_TRIF = 357
SM_TRIB = 485
SM_ID = 613
NS = 741


class Ctx:
    pass


def build_program(debug=None, phases=(0, 1, 2, 3, 4)):
    nc = bass.Bass("TRN2", target_bir_lowering=False)
    K = Ctx()
    K.nc = nc
    K.debug = debug or ()

    def din(name, shape, dt=F32):
        return nc.dram_tensor(name, list(shape), dt, kind="ExternalInput").ap()

    def dscr(name, shape, dt):
        kind = "ExternalOutput" if name in K.debug else "Internal"
        return nc.dram_tensor(name, list(shape), dt, kind=kind).ap()

    K.x = din("x_loc", [T, D])
    K.ctx = din("ctx_loc", [NCTX, D])
    K.w_in = din("w_loc", [D, WCOLS])
    K.w_ada = din("w_ada", [D, 6 * D])
    K.w_out = din("w_out", [D, D])
    K.w_fc1 = din("w_fc1", [D, DFF])
    K.w_fc2 = din("w_fc2", [DFF, D])
    K.cosT = din("cosT", [128, T])
    K.sinT = din("sinT", [128, T])
    K.smalls = din("smalls", [128, NS])
    K.bc_in = din("bc_in", [128, 3, D])
    K.out = nc.dram_tensor("out", [TOWN, D], F32, kind="ExternalOutput").ap()
    K.QT = dscr("QT", [4, 128, TOWN], BF16)
    K.KT = dscr("KT", [4, 128, NKEY], BF16)
    K.V = dscr("V", [NKEY, 512], BF16)
    K.MV = dscr("MV", [NKEY, 512], BF16)
    K.MK = dscr("MK", [NKEY, 256], BF16)
    K.G = dscr("G", [NKEY, 16], F32)
    K.MQT = dscr("MQT", [2, 128, TOWN], BF16)
    K.MKT = dscr("MKT", [2, 128, TOWN], BF16)
    K.MO = dscr("MO", [TOWN, 512], BF16)
    K.MIXT = dscr("MIXT", [8, 128, TOWN], BF16)
    K.DBG = dscr("DBG", [128, 2048], F32)

    with ExitStack() as es:
        S = Sched(nc, es)
        K.S = S
        K.tok = {n: Tok(n) for n in ("QT", "KT", "V", "MV", "MK", "G", "MQT", "MKT", "MO", "MIXT")}
        phase0(K)
        if 1 in phases:
            phase1(K)
        if 2 in phases:
            phase2(K)
        if 3 in phases:
            phase3(K)
        if 4 in phases:
            phase4(K)
        S.finish()
    return nc


def phase0(K):
    S = K.S
    sm = K.sm = S.sb("sm", [128, NS], F32)
    S.dma("sp", sm.t[:, :], K.smalls, writes=[sm])
    K.identB = S.sb("identB", [128, 128], BF16)
    K.onesF = S.sb("onesF", [128, 128], F32)
    K.onesB = S.sb("onesB", [128, 128], BF16)
    K.modfm = S.sb("modfm", [128, 48, 2], F32)
    K.vec = S.sb("vec", [128, 64], F32)
    K.bc = S.sb("bc", [128, 3, D], F32)
    S.dma("sp", K.bc.t[:, :, :], K.bc_in, writes=[K.bc])
    S.op("dve", lambda e: e.tensor_copy(K.identB.t[:, :], sm.t[:, SM_ID:SM_ID + 128]), reads=[sm], writes=[K.identB])
    S.op("dve", lambda e: e.memset(K.onesF.t[:, :], 1.0), writes=[K.onesF])
    S.op("dve", lambda e: e.memset(K.onesB.t[:, :], 1.0), writes=[K.onesB])
    with ExitStack() as mes:
        S.mes = mes
        sc = S.sb("sc", [128, 16], F32)
        screp = S.sb("screp", [128, 8, 128], F32)
        wa = [S.sb("wa0", [128, 8, 1024], F32), S.sb("wa1", [128, 8, 1024], F32)]
        psm = S.ps("psm", [128, 96], F32)
        psg = [S.ps("psg0", [128, 512], F32), S.ps("psg1", [128, 512], F32)]
        S.op("act", lambda e: e.activation(sc.t[:, :], sm.t[:, SM_CT:SM_CT + 16], AF.Silu), reads=[sm], writes=[sc])
        for kc in range(8):
            S.op("dve", lambda e, kc=kc: e.tensor_scalar(screp.t[:, kc, :], K.onesF.t[:, :], sc.t[:, 2 * kc:2 * kc + 1], None, ALU.mult),
                 reads=[K.onesF, sc], writes=[screp])
        for piece in range(6):
            w = wa[piece % 2]
            S.dma("sp", w.t[:, :, :], K.w_ada[:, piece * D:(piece + 1) * D].rearrange("(k p) f -> p k f", p=128), writes=[w])
            for fcl in range(8):
                fc = piece * 8 + fcl
                for kc in range(8):
                    S.op("pe", lambda e, w=w, fc=fc, fcl=fcl, kc=kc: e.matmul(
                        psm.t[:, 2 * fc:2 * fc + 2], w.t[:, kc, fcl * 128:(fcl + 1) * 128],
                        sc.t[:, 2 * kc:2 * kc + 2], start=(kc == 0), stop=(kc == 7)),
                        reads=[w, sc], writes=[psm], inc=(kc == 7))
            if piece in (2, 5):
                gi = 0 if piece == 2 else 1
                for half in range(2):
                    pg = psg[half]
                    for kc in range(8):
                        S.op("pe", lambda e, w=w, pg=pg, half=half, kc=kc: e.matmul(
                            pg.t[:, :], screp.t[:, kc, :], w.t[:, kc, half * 512:(half + 1) * 512],
                            start=(kc == 0), stop=(kc == 7)), reads=[w, screp], writes=[pg], inc=(kc == 7))
                    S.op("dve", lambda e, pg=pg, gi=gi, half=half: e.tensor_tensor(
                        K.bc.t[:, gi, half * 512:(half + 1) * 512], pg.t[:, :],
                        K.bc.t[:, gi, half * 512:(half + 1) * 512], ALU.add), reads=[pg, K.bc], writes=[K.bc])
        pm3 = psm.t[:, :].rearrange("p (f j) -> p f j", j=2)
        for j in range(2):
            S.op("dve", lambda e, j=j: e.tensor_tensor(K.modfm.t[:, :, j], pm3[:, :, j], sm.t[:, SM_BADA:SM_BADA + 48], ALU.add),
                 reads=[psm, sm], writes=[K.modfm])
        v = K.vec
        mf = K.modfm
        n1g = sm.t[:, SM_NG:SM_NG + 8]
        n2g = sm.t[:, SM_NG + 8:SM_NG + 16]
        S.op("dve", lambda e: e.scalar_tensor_tensor(v.t[:, 0:8], mf.t[:, 8:16, 0], 1.0, n1g, ALU.add, ALU.mult), reads=[mf, sm], writes=[v])
        S.op("dve", lambda e: e.scalar_tensor_tensor(v.t[:, 8:16], mf.t[:, 8:16, 1], 1.0, n1g, ALU.add, ALU.mult), reads=[mf, sm], writes=[v])
        S.op("dve", lambda e: e.scalar_tensor_tensor(v.t[:, 16:24], mf.t[:, 32:40, 0], 1.0, n2g, ALU.add, ALU.mult), reads=[mf, sm], writes=[v])
        lt = S.sb("lt", [128, 128], F32)
        S.op("dve", lambda e: e.tensor_tensor(lt.t[:, 0:64], sm.t[:, SM_LAM:SM_LAM + 64], sm.t[:, SM_LAM + 64:SM_LAM + 128], ALU.mult), reads=[sm], writes=[lt])
        S.op("dve", lambda e: e.tensor_tensor(lt.t[:, 64:128], sm.t[:, SM_LAM + 128:SM_LAM + 192], sm.t[:, SM_LAM + 192:SM_LAM + 256], ALU.mult), reads=[sm], writes=[lt])
        S.op("dve", lambda e: e.reduce_sum(v.t[:, 27:28], lt.t[:, 0:64], AX.X), reads=[lt], writes=[v])
        S.op("dve", lambda e: e.reduce_sum(v.t[:, 28:29], lt.t[:, 64:128], AX.X), reads=[lt], writes=[v])
        S.op("act", lambda e: e.activation(v.t[:, 29:31], v.t[:, 27:29], AF.Exp), reads=[v], writes=[v])
        S.op("dve", lambda e: e.tensor_tensor(v.t[:, 24:25], v.t[:, 29:30], v.t[:, 30:31], ALU.subtract), reads=[v], writes=[v])
        S.op("dve", lambda e: e.tensor_scalar(v.t[:, 24:25], v.t[:, 24:25], 0.2, None, ALU.add), reads=[v], writes=[v])
        S.op("dve", lambda e: e.tensor_scalar(v.t[:, 25:26], v.t[:, 24:25], -1.0, None, ALU.mult), reads=[v], writes=[v])
        S.op("dve", lambda e: e.tensor_scalar(v.t[:, 26:27], sm.t[:, SM_SUB:SM_SUB + 1], 0.8, None, ALU.mult), reads=[sm], writes=[v])
        if "DBG" in K.debug:
            dbg = S.sb("dbg", [128, 2048], F32)
            S.op("dve", lambda e: e.memset(dbg.t[:, :], 0.0), writes=[dbg])
            S.op("dve", lambda e: e.tensor_copy(dbg.t[:, 0:96], mf.t[:, :, :].rearrange("p f j -> p (f j)")), reads=[mf], writes=[dbg])
            S.op("dve", lambda e: e.tensor_copy(dbg.t[:, 96:160], v.t[:, :]), reads=[v], writes=[dbg])
            S.op("dve", lambda e: e.tensor_copy(dbg.t[:, 1024:2048], K.bc.t[:, 0, :]), reads=[K.bc], writes=[dbg])
            S.dma("sp", K.DBG, dbg.t[:, :], reads=[dbg], out=True)
        S.flush()
    S.mes = S.es


def phase1(K):
    S = K.S
    sm, v, mf = K.sm, K.vec, K.modfm
    with ExitStack() as mes:
        S.mes = mes
        Wb = S.sb("Wb", [128, 8, WCOLS], BF16)
        for kc in range(8):
            S.dma("pool", Wb.t[:, kc, :], K.w_in[kc * 128:(kc + 1) * 128, :], writes=[Wb])
        NB = 2
        xt = [S.sb("xt", [128, 4, D], F32)] * NB
        xh = [S.sb("xh", [128, 4, D], BF16)] * NB
        xm = [S.sb(f"xm{i}", [128, 8, 512], BF16) for i in range(NB)]
        st = [S.sb(f"st{i}", [128, 16], F32) for i in range(NB)]
        junk = S.sb("junk", [128, D], BF16)
        cs = [S.sb(f"cs{i}", [128, 2, 512], F32) for i in range(NB)]
        qk = [S.sb("qk", [128, 8, 512], BF16)] * NB
        t1 = [S.sb(f"t1{i}", [128, 512], F32) for i in range(2)]
        t2 = [S.sb(f"t2{i}", [128, 512], F32) for i in range(2)]
        fmo = [S.sb("fmo", [128, 4, 512], BF16)] * NB
        tmo = [S.sb("tmo", [128, 4, 1792], BF16)] * NB
        gto = [S.sb("gto", [128, 4, 16], F32)] * NB
        pst = [S.ps(f"pst{i}", [128, 2, 512], BF16) for i in range(2)]
        psf = [S.ps(f"psf{i}", [128, 512], F32) for i in range(4)]
        pso = [S.ps(f"pso{i}", [128, 512], F32) for i in range(2)]
        nps = 0
        npo = 0
        blocks = [("own", i) for i in range(8)] + [("other", i) for i in range(8)] + [("ctx", 0)]
        def blk_geom(bi):
            kind, ib = blocks[bi]
            nt = 2 if kind == "ctx" else 4
            if kind == "ctx":
                tok0 = T
            else:
                tok0 = ib * 512 + (TOWN if kind == "other" else 0)
            return kind, nt, tok0

        def prep(bi):
            kind, nt, tok0 = blk_geom(bi)
            p = bi % NB
            if kind == "ctx":
                src = K.ctx.rearrange("(i p) d -> p i d", p=128)
            else:
                src = K.x[tok0:tok0 + 512, :].rearrange("(i p) d -> p i d", p=128)
            S.dma("sp", xt[p].t[:, 0:nt, :], src, writes=[xt[p]])
            if kind != "ctx":
                S.dma("sp", cs[p].t[:, 0, :], K.cosT[:, tok0:tok0 + 512], writes=[cs[p]])
                S.dma("sp", cs[p].t[:, 1, :], K.sinT[:, tok0:tok0 + 512], writes=[cs[p]])
            for i in range(nt):
                S.op("act", lambda e, p=p, i=i: e.activation(junk.t[:, :], xt[p].t[:, i, :], AF.Square, accum_out=st[p].t[:, i:i + 1]),
                     reads=[xt[p]], writes=[junk, st[p]])
            S.op("act", lambda e, p=p, nt=nt: e.activation(st[p].t[:, 4:4 + nt], st[p].t[:, 0:nt], AF.Ln, bias=EPS, scale=1.0 / D),
                 reads=[st[p]], writes=[st[p]])
            S.op("act", lambda e, p=p, nt=nt: e.activation(st[p].t[:, 8:8 + nt], st[p].t[:, 4:4 + nt], AF.Exp, scale=-0.5),
                 reads=[st[p]], writes=[st[p]])
            for i in range(nt):
                S.op("dve", lambda e, p=p, i=i: e.tensor_scalar(xh[p].t[:, i, :], xt[p].t[:, i, :], st[p].t[:, 8 + i:9 + i], None, ALU.mult),
                     reads=[xt[p], st[p]], writes=[xh[p]])

        prep(0)
        for bi, (kind, ib) in enumerate(blocks):
            p = bi % NB
            kind, nt, tok0 = blk_geom(bi)
            ntok = nt * 128
            acol = 8 if kind == "ctx" else 0
            bj = 1 if kind == "ctx" else 0
            for k2 in range(4):
                pt = pst[k2 % 2]
                for kk in range(2):
                    kc = 2 * k2 + kk
                    for i in range(nt):
                        S.op("pe", lambda e, pt=pt, kk=kk, i=i, p=p, kc=kc: e.transpose(
                            pt.t[:, kk, i * 128:(i + 1) * 128], xh[p].t[:, i, kc * 128:(kc + 1) * 128], K.identB.t[:, :]),
                            reads=[xh[p], K.identB], writes=[pt], inc=(kk == 1 and i == nt - 1))
                for kk in range(2):
                    kc = 2 * k2 + kk
                    eng = "act" if kk == 0 else "dve"
                    if eng == "act":
                        S.op("act", lambda e, pt=pt, kk=kk, p=p, kc=kc, ntok=ntok, acol=acol, bj=bj: e.activation(
                            xm[p].t[:, kc, 0:ntok], pt.t[:, kk, 0:ntok], AF.Identity,
                            scale=v.t[:, acol + kc:acol + kc + 1], bias=mf.t[:, kc, bj:bj + 1]),
                            reads=[pt, v, mf], writes=[xm[p]])
                    else:
                        S.op("dve", lambda e, pt=pt, kk=kk, p=p, kc=kc, ntok=ntok, acol=acol, bj=bj: e.tensor_scalar(
                            xm[p].t[:, kc, 0:ntok], pt.t[:, kk, 0:ntok],
                            v.t[:, acol + kc:acol + kc + 1], mf.t[:, kc, bj:bj + 1], ALU.mult, ALU.add),
                            reads=[pt, v, mf], writes=[xm[p]])

            if bi + 1 < len(blocks):
                prep(bi + 1)

            def fm_mm(ps, chunk, p=p, ntok=ntok):
                for kc in range(8):
                    S.op("pe", lambda e, kc=kc: e.matmul(ps.t[:, 0:ntok], Wb.t[:, kc, chunk * 128:(chunk + 1) * 128],
                                                        xm[p].t[:, kc, 0:ntok], start=(kc == 0), stop=(kc == 7)),
                         reads=[Wb, xm[p]], writes=[ps], inc=(kc == 7))

            for h in range(4):
                for which in ((0, 1) if kind == "own" else (1,)):
                    base = 4 * h + 2 * which
                    slot = 2 * h + which
                    if kind == "ctx":
                        ps = psf[nps % 4]; nps += 1
                        fm_mm(ps, base)
                        S.op("act", lambda e, ps=ps, p=p, slot=slot, ntok=ntok: e.copy(qk[p].t[:, slot, 0:ntok], ps.t[:, 0:ntok]),
                             reads=[ps], writes=[qk[p]])
                        continue
                    psa = psf[nps % 4]; nps += 1
                    psb = psf[nps % 4]; nps += 1
                    fm_mm(psa, base)
                    fm_mm(psb, base + 1)
                    ta = t1[slot % 2]
                    tb = t2[slot % 2]
                    S.op("dve", lambda e, psa=psa, ta=ta, p=p: e.tensor_tensor(ta.t[:, :], psa.t[:, :], cs[p].t[:, 0, :], ALU.mult),
                         reads=[psa, cs[p]], writes=[ta])
                    S.op("dve", lambda e, psb=psb, tb=tb, p=p: e.tensor_tensor(tb.t[:, :], psb.t[:, :], cs[p].t[:, 1, :], ALU.mult),
                         reads=[psb, cs[p]], writes=[tb])
                    S.op("pool", lambda e, ta=ta, tb=tb, p=p, slot=slot: e.tensor_tensor(qk[p].t[:, slot, :], ta.t[:, :], tb.t[:, :], ALU.add),
                         reads=[ta, tb], writes=[qk[p]])
            for h in range(4):
                if kind == "own":
                    S.dma("sp", K.QT[h, :, tok0:tok0 + 512], qk[p].t[:, 2 * h, :], reads=[qk[p]])
                S.dma("sp", K.KT[h, :, tok0:tok0 + ntok], qk[p].t[:, 2 * h + 1, 0:ntok], reads=[qk[p]])
            if kind == "own":
                for c4 in range(4):
                    ps = psf[nps % 4]; nps += 1
                    fm_mm(ps, 16 + c4)
                    if c4 < 2:
                        S.op("act", lambda e, ps=ps, p=p, c4=c4: e.copy(fmo[p].t[:, c4, :], ps.t[:, :]), reads=[ps], writes=[fmo[p]])
                    else:
                        S.op("act", lambda e, ps=ps, p=p, c4=c4: e.mul(fmo[p].t[:, c4, :], ps.t[:, :], 0.125), reads=[ps], writes=[fmo[p]])
                for c2 in range(2):
                    S.dma("sp", K.MQT[c2, :, tok0:tok0 + 512], fmo[p].t[:, c2, :], reads=[fmo[p]])
                    S.dma("sp", K.MKT[c2, :, tok0:tok0 + 512], fmo[p].t[:, 2 + c2, :], reads=[fmo[p]])
            for i in range(nt):
                groups = [(C_DV, 512, 0), (C_MV, 512, 512)]
                if kind == "own":
                    groups.append((C_MO, 512, 1024))
                groups.append((C_MK, 272, 1536))
                for (c0, n, o0) in groups:
                    ps = pso[npo % 2]; npo += 1
                    for kc in range(8):
                        S.op("pe", lambda e, ps=ps, kc=kc, i=i, c0=c0, n=n, p=p: e.matmul(
                            ps.t[:, 0:n], xm[p].t[:, kc, i * 128:(i + 1) * 128], Wb.t[:, kc, c0:c0 + n],
                            start=(kc == 0), stop=(kc == 7)), reads=[Wb, xm[p]], writes=[ps], inc=(kc == 7))
                    if c0 == C_MO:
                        S.op("act", lambda e, ps=ps, p=p, i=i, o0=o0: e.activation(tmo[p].t[:, i, o0:o0 + 512], ps.t[:, :], AF.Sigmoid),
                             reads=[ps], writes=[tmo[p]])
                    elif c0 == C_MK:
                        S.op("dve", lambda e, ps=ps, p=p, i=i, o0=o0: e.tensor_scalar(tmo[p].t[:, i, o0:o0 + 256], ps.t[:, 0:256], 0.125, None, ALU.mult),
                             reads=[ps], writes=[tmo[p]])
                        S.op("dve", lambda e, ps=ps, p=p, i=i: e.tensor_tensor(gto[p].t[:, i, :], ps.t[:, 256:272], sm.t[:, SM_BG:SM_BG + 16], ALU.add),
                             reads=[ps, sm], writes=[gto[p]])
                    elif c0 == C_DV:
                        S.op("dve", lambda e, ps=ps, p=p, i=i, o0=o0: e.tensor_copy(tmo[p].t[:, i, o0:o0 + 512], ps.t[:, :]),
                             reads=[ps], writes=[tmo[p]])
                    else:
                        S.op("act", lambda e, ps=ps, p=p, i=i, o0=o0: e.copy(tmo[p].t[:, i, o0:o0 + 512], ps.t[:, :]),
                             reads=[ps], writes=[tmo[p]])
            rows = slice(tok0, tok0 + ntok)
            S.dma("sp", K.V[rows, :].rearrange("(i p) c -> p i c", p=128), tmo[p].t[:, 0:nt, 0:512], reads=[tmo[p]])
            S.dma("sp", K.MV[rows, :].rearrange("(i p) c -> p i c", p=128), tmo[p].t[:, 0:nt, 512:1024], reads=[tmo[p]])
            if kind == "own":
                S.dma("sp", K.MO[rows, :].rearrange("(i p) c -> p i c", p=128), tmo[p].t[:, 0:nt, 1024:1536], reads=[tmo[p]])
            S.dma("sp", K.MK[rows, :].rearrange("(i p) c -> p i c", p=128), tmo[p].t[:, 0:nt, 1536:1792], reads=[tmo[p]])
            S.dma("sp", K.G[rows, :].rearrange("(i p) c -> p i c", p=128), gto[p].t[:, 0:nt, :], reads=[gto[p]])
        S.flush()
    S.mes = S.es


def phase2(K):
    S = K.S
    sm = K.sm
    with ExitStack() as mes:
        S.mes = mes
        RS = S.sb("RS", [128, NT, 4, 2], F32)
        WW = S.sb("WW", [128, NT, 4, 2], F32)
        CS = S.sb("CS", [128, NT, 4, 2], F32)
        EE = S.sb("EE", [128, NT, 4, 2], F32)
        with ExitStack() as sub:
            S.mes = sub
            G = S.sb("G", [128, NT, 16], F32)
            S.dma("sp", G.t[:, :, :], K.G.rearrange("(j p) g -> p j g", p=128), writes=[G])
            spl = S.sb("spl", [128, NT, 16], F32)
            S.op("act", lambda e: e.activation(spl.t[:, :, :], G.t[:, :, :], AF.Exp, scale=-1.0), reads=[G], writes=[spl])
            S.op("act", lambda e: e.activation(spl.t[:, :, :], spl.t[:, :, :], AF.Ln, bias=1.0), reads=[spl], writes=[spl])
            spc = S.sb("spc", [128, NT, 4], F32)
            a1 = S.sb("a1", [128, NT, 4], F32)
            a2 = S.sb("a2", [128, NT, 4], F32)
            psc = S.ps("psc", [128, 512], F32)
            pst_ = S.ps("pstot", [128, 512], F32)
            NN = NT * 4
            for d in range(2):
                tri = sm.t[:, SM_TRIF:SM_TRIF + 128] if d == 0 else sm.t[:, SM_TRIB:SM_TRIB + 128]
                g0 = 8 * d
                S.op("dve", lambda e, g0=g0: e.tensor_copy(spc.t[:, :, :], spl.t[:, :, g0 + 4:g0 + 8]), reads=[spl], writes=[spc])
                spc2 = spc.t[:, :, :].rearrange("p j h -> p (j h)")
                S.op("pe", lambda e, tri=tri, spc2=spc2: e.matmul(psc.t[:, 0:NN], tri, spc2, start=True, stop=True), reads=[sm, spc], writes=[psc])
                S.op("pe", lambda e, spc2=spc2: e.matmul(pst_.t[:, 0:NN], K.onesF.t[:, :], spc2, start=True, stop=True), reads=[K.onesF, spc], writes=[pst_])
                cs3 = psc.t[:, 0:NN].rearrange("p (j h) -> p j h", h=4)
                tot3 = pst_.t[:, 0:NN].rearrange("p (j h) -> p j h", h=4)
                S.op("dve", lambda e, cs3=cs3, g0=g0: e.tensor_tensor(a1.t[:, :, :], cs3, G.t[:, :, g0:g0 + 4], ALU.add), reads=[psc, G], writes=[a1])
                S.op("dve", lambda e, tot3=tot3: e.tensor_tensor(a2.t[:, :, :], a1.t[:, :, :], tot3, ALU.subtract), reads=[pst_, a1], writes=[a2])
                S.op("act", lambda e, d=d: e.activation(RS.t[:, :, :, d], a1.t[:, :, :], AF.Exp), reads=[a1], writes=[RS])
                S.op("act", lambda e, d=d: e.activation(WW.t[:, :, :, d], a2.t[:, :, :], AF.Exp), reads=[a2], writes=[WW])
                S.op("act", lambda e, d=d, cs3=cs3: e.activation(CS.t[:, :, :, d], cs3, AF.Exp, scale=-1.0), reads=[psc], writes=[CS])
                S.op("act", lambda e, d=d, tot3=tot3: e.activation(EE.t[:, :, :, d], tot3, AF.Exp, scale=-1.0), reads=[pst_], writes=[EE])
            S.flush()
        S.mes = mes
        mk_tok = S.sb("mk_tok", [128, NT, 64], BF16)
        v_aug = S.sb("v_aug", [128, NT, 129], BF16)
        mqT = S.sb("mqT", [64, TOWN], BF16)
        mkT = S.sb("mkT", [64, TOWN], BF16)
        mo_h = S.sb("mo_h", [128, 32, 128], BF16)
        kw = [S.sb("kwF", [128, NT, 64], BF16), S.sb("kwR", [128, NT, 64], BF16)]
        Cs = [S.sb("CsF", [64, 34, 129], F32), S.sb("CsR", [64, 34, 129], F32)]
        Cb = [S.sb("CbF", [64, 32, 129], BF16), S.sb("CbR", [64, 32, 129], BF16)]
        Pm = [[S.sb(f"P{d}{i}", [128, 128], BF16) for i in range(2)] for d in range(2)]
        sml = [S.sb(f"sml{i}", [128, 16], F32) for i in range(2)]
        hF = [S.sb(f"hF{i}", [128, 128], F32) for i in range(2)]
        hs = [S.sb(f"hs{i}", [128, 128], F32) for i in range(2)]
        junk = S.sb("junk2", [128, 128], BF16)
        mlb = [S.sb(f"mlb{i}", [128, 128], BF16) for i in range(2)]
        mlT = [S.sb(f"mlT{i}", [128, 4, 128], BF16) for i in range(2)]
        psU = [S.ps(f"psU{i}", [128, 512], F32) for i in range(2)]
        psS = [S.ps(f"psS{i}", [128, 128], F32) for i in range(2)]
        psH = [S.ps(f"psH{i}", [128, 2, 129], F32) for i in range(2)]
        psT = [S.ps(f"psT{i}", [128, 128], BF16) for i in range(2)]
        S.op("pool", lambda e: e.memset(v_aug.t[:, :, 128:129], 1.0), writes=[v_aug])
        seqs = [[64, 65] + list(range(0, 31)), [65, 64] + list(range(63, 31, -1)) + list(range(31, 0, -1))]
        nU = 0
        for h in range(4):
            S.dma("sp", mk_tok.t[:, :, :], K.MK[:, h * 64:(h + 1) * 64].rearrange("(j p) d -> p j d", p=128), writes=[mk_tok])
            S.dma("sp", v_aug.t[:, :, 0:128], K.MV[:, h * 128:(h + 1) * 128].rearrange("(j p) d -> p j d", p=128), writes=[v_aug])
            r0 = (h % 2) * 64
            S.dma("sp", mqT.t[:, :], K.MQT[h // 2, r0:r0 + 64, :], writes=[mqT])
            S.dma("sp", mkT.t[:, :], K.MKT[h // 2, r0:r0 + 64, :], writes=[mkT])
            S.dma("sp", mo_h.t[:, :, :], K.MO[:, h * 128:(h + 1) * 128].rearrange("(j p) d -> p j d", p=128), writes=[mo_h])
            for d in range(2):
                S.op("dve", lambda e, d=d, h=h: e.tensor_tensor(kw[d].t[:, :, :], mk_tok.t[:, :, :],
                                                             WW.t[:, :, h, d:d + 1].to_broadcast([128, NT, 64]), ALU.mult),
                     reads=[mk_tok, WW], writes=[kw[d]])
            for d in range(2):
                seq = seqs[d]
                npre = len(seq) - 31
                prev = None
                for si, j in enumerate(seq):
                    u = si % 3
                    if u == 0:
                        pu = psU[nU % 2]; nU += 1
                        grp = seq[si:si + 3]
                        for gi, jj in enumerate(grp):
                            S.op("pe", lambda e, pu=pu, gi=gi, jj=jj, d=d: e.matmul(
                                pu.t[0:64, gi * 129:(gi + 1) * 129], kw[d].t[:, jj, :], v_aug.t[:, jj, :], start=True, stop=True),
                                reads=[kw[d], v_aug], writes=[pu], inc=(gi == len(grp) - 1))
                    if si < npre - 1:
                        dst = si % 2
                    else:
                        dst = 2 + (si - (npre - 1))
                    uap = pu.t[0:64, u * 129:(u + 1) * 129]
                    if prev is None:
                        S.op("dve", lambda e, d=d, dst=dst, uap=uap: e.tensor_copy(Cs[d].t[:, dst, :], uap), reads=[pu], writes=[Cs[d]])
                    else:
                        S.op("dve", lambda e, d=d, dst=dst, uap=uap, prev=prev, j=j, h=h: e.scalar_tensor_tensor(
                            Cs[d].t[:, dst, :], Cs[d].t[:, prev, :], EE.t[0:64, j, h, d:d + 1], uap, ALU.mult, ALU.add),
                            reads=[pu, Cs[d], EE], writes=[Cs[d]])
                    prev = dst
                S.op("pool", lambda e, d=d: e.tensor_copy(Cb[d].t[:, :, :], Cs[d].t[:, 2:34, :]), reads=[Cs[d]], writes=[Cb[d]])
            for j in range(32):
                q = j % 2
                tsl = slice(j * 128, (j + 1) * 128)
                S.op("pe", lambda e, q=q, tsl=tsl: e.matmul(psS[q].t[:, :], mkT.t[:, tsl], mqT.t[:, tsl], start=True, stop=True),
                     reads=[mkT, mqT], writes=[psS[q]])
                for d in range(2):
                    tri = sm.t[:, SM_TRIF:SM_TRIF + 128] if d == 0 else sm.t[:, SM_TRIB:SM_TRIB + 128]
                    S.op("dve", lambda e, d=d, q=q, j=j, h=h, tri=tri: e.scalar_tensor_tensor(
                        Pm[d][q].t[:, :], psS[q].t[:, :], RS.t[:, j, h, d:d + 1], tri, ALU.mult, ALU.mult),
                        reads=[psS[q], RS, sm], writes=[Pm[d][q]])
                for d in range(2):
                    slot = j if d == 0 else 31 - j
                    S.op("pe", lambda e, d=d, q=q, j=j: e.matmul(psH[q].t[:, d, :], Pm[d][q].t[:, :], v_aug.t[:, j, :], start=True, stop=False),
                         reads=[Pm[d][q], v_aug], writes=[psH[q]], inc=False)
                    S.op("pe", lambda e, d=d, q=q, tsl=tsl, slot=slot: e.matmul(psH[q].t[:, d, :], mqT.t[:, tsl], Cb[d].t[:, slot, :], start=False, stop=True),
                         reads=[mqT, Cb[d]], writes=[psH[q]], inc=(d == 1))
                sq = sml[q]
                S.op("dve", lambda e, q=q, sq=sq, j=j, h=h: e.tensor_tensor(sq.t[:, 0:2], psH[q].t[:, :, 128], CS.t[:, j, h, :], ALU.mult),
                     reads=[psH[q], CS], writes=[sq])
                S.op("dve", lambda e, sq=sq: e.scalar_tensor_tensor(sq.t[:, 2:4], sq.t[:, 0:2], -1.0, sq.t[:, 0:2], ALU.mult, ALU.max), reads=[sq], writes=[sq])
                S.op("dve", lambda e, sq=sq: e.tensor_scalar(sq.t[:, 2:4], sq.t[:, 2:4], 1.0, None, ALU.max), reads=[sq], writes=[sq])
                S.op("dve", lambda e, sq=sq: e.reciprocal(sq.t[:, 4:6], sq.t[:, 2:4]), reads=[sq], writes=[sq])
                S.op("dve", lambda e, sq=sq, j=j, h=h: e.tensor_tensor(sq.t[:, 6:8], sq.t[:, 4:6], CS.t[:, j, h, :], ALU.mult), reads=[sq, CS], writes=[sq])
                S.op("dve", lambda e, q=q, sq=sq: e.tensor_scalar(hF[q].t[:, :], psH[q].t[:, 0, 0:128], sq.t[:, 6:7], None, ALU.mult),
                     reads=[psH[q], sq], writes=[hF[q]])
                S.op("dve", lambda e, q=q, sq=sq: e.scalar_tensor_tensor(hs[q].t[:, :], psH[q].t[:, 1, 0:128], sq.t[:, 7:8], hF[q].t[:, :], ALU.mult, ALU.add),
                     reads=[psH[q], sq, hF[q]], writes=[hs[q]])
                S.op("act", lambda e, q=q, sq=sq: e.activation(junk.t[:, :], hs[q].t[:, :], AF.Square, accum_out=sq.t[:, 8:9]),
                     reads=[hs[q]], writes=[junk, sq])
                S.op("act", lambda e, sq=sq: e.activation(sq.t[:, 9:10], sq.t[:, 8:9], AF.Ln, bias=EPS, scale=1.0 / 128), reads=[sq], writes=[sq])
                S.op("act", lambda e, sq=sq: e.activation(sq.t[:, 10:11], sq.t[:, 9:10], AF.Exp, scale=-0.5), reads=[sq], writes=[sq])
                S.op("dve", lambda e, q=q, sq=sq, j=j: e.scalar_tensor_tensor(mlb[q].t[:, :], hs[q].t[:, :], sq.t[:, 10:11], mo_h.t[:, j, :], ALU.mult, ALU.mult),
                     reads=[hs[q], sq, mo_h], writes=[mlb[q]])
                S.op("pe", lambda e, q=q: e.transpose(psT[q].t[:, :], mlb[q].t[:, :], K.identB.t[:, :]), reads=[mlb[q], K.identB], writes=[psT[q]])
                mt = mlT[(j // 4) % 2]
                S.op("act", lambda e, q=q, mt=mt, j=j: e.copy(mt.t[:, j % 4, :], psT[q].t[:, :]), reads=[psT[q]], writes=[mt])
                if j % 4 == 3:
                    j0 = j - 3
                    S.dma("sp", K.MIXT[4 + h, :, j0 * 128:(j0 + 4) * 128], mt.t[:, :, :].rearrange("p a b -> p (a b)"), reads=[mt])
        S.flush()
    S.mes = S.es


def phase3(K):
    S = K.S
    v = K.vec
    NQB = TOWN // 512
    with ExitStack() as mes:
        S.mes = mes
        KTh = [S.sb(f"KTh{i}", [128, NKEY], BF16) for i in range(2)]
        Vh = [S.sb(f"Vh{i}", [128, NT, 128], BF16) for i in range(2)]
        QTh = [S.sb(f"QTh{i}", [128, TOWN], BF16) for i in range(2)]
        NE = 4
        E = [S.sb(f"E{i}", [128, 1024], BF16) for i in range(NE)]
        sel1 = S.sb("sel1", [128, 128], F32)
        sel2 = S.sb("sel2", [128, 128], F32)
        zs = S.sb("zs", [128, 512], F32)
        idf = K.sm.t[:, SM_ID:SM_ID + 128]
        S.op("dve", lambda e: e.tensor_tensor(sel1.t[:, :], idf[:, 0:1].to_broadcast([128, 128]), idf[:, 64:65].to_broadcast([128, 128]), ALU.add),
             reads=[K.sm], writes=[sel1])
        S.op("dve", lambda e: e.tensor_tensor(sel2.t[:, :], idf[:, 32:33].to_broadcast([128, 128]), idf[:, 96:97].to_broadcast([128, 128]), ALU.add),
             reads=[K.sm], writes=[sel2])
        S.op("dve", lambda e: e.tensor_tensor(sel1.t[:, :], sel1.t[:, :], sel2.t[:, :], ALU.add), reads=[sel1, sel2], writes=[sel1])
        acc2 = [S.sb(f"acc2_{i}", [128, 512], F32) for i in range(2)]
        rr = S.sb("rr", [128, 1024], F32)
        o1 = S.sb("o1", [128, 512], F32)
        o2 = S.sb("o2", [128, 512], F32)
        oo = S.sb("oo", [128, 512], F32)
        sq = S.sb("sq", [128, 512], F32)
        lnv = S.sb("lnv", [128, 512], F32)
        rstd = S.sb("rstd", [128, 512], F32)
        dao = [S.sb(f"dao{i}", [128, 512], BF16) for i in range(2)]
        psS = [S.ps(f"psS{i}", [128, 1024], F32) for i in range(2)]
        psO = S.ps("psO", [128, 1024], F32)
        psZ = S.ps("psZ", [128, 512], F32)
        psB = S.ps("psB", [128, 512], F32)

        def load_head(h):
            b = h % 2
            S.dma("sp", KTh[b].t[:, :], K.KT[h, :, :], writes=[KTh[b]])
            S.dma("sp", Vh[b].t[:, :, :], K.V[:, h * 128:(h + 1) * 128].rearrange("(j p) d -> p j d", p=128), writes=[Vh[b]])
            S.dma("sp", QTh[b].t[:, :], K.QT[h, :, :], writes=[QTh[b]])

        pendB = []

        def attn_block(h, b, qb, it, nq):
            qs = slice(qb * 512, (qb + 1) * 512)
            ac2 = acc2[nq % 2]

            def qk(kb, itn):
                ps = psS[itn % 2]
                ks = slice(kb * 128, (kb + 1) * 128)
                S.op("pe", lambda e: e.matmul(ps.t[:, 0:512], KTh[b].t[0:64, ks], QTh[b].t[0:64, qs], start=True, stop=True),
                     reads=[KTh[b], QTh[b]], writes=[ps], inc=False)
                S.op("pe", lambda e: e.matmul(ps.t[:, 512:1024], KTh[b].t[64:128, ks], QTh[b].t[64:128, qs], start=True, stop=True),
                     reads=[KTh[b], QTh[b]], writes=[ps])

            qk(0, it)
            for kb in range(NT):
                itn = it + kb
                if kb + 1 < NT:
                    qk(kb + 1, itn + 1)
                if kb == 6 and pendB:
                    pendB.pop(0)()
                ps = psS[itn % 2]
                Et = E[itn % NE]
                S.op("act", lambda e, ps=ps, Et=Et: e.activation(Et.t[:, :], ps.t[:, :], AF.Exp, scale=0.125), reads=[ps], writes=[Et])
                S.op("pe", lambda e, Et=Et, kb=kb: e.matmul(psO.t[:, 0:512], Vh[b].t[:, kb, :], Et.t[:, 0:512], start=(kb == 0), stop=(kb == NT - 1)),
                     reads=[Vh[b], Et], writes=[psO], inc=False)
                S.op("pe", lambda e, Et=Et, kb=kb: e.matmul(psO.t[:, 512:1024], Vh[b].t[:, kb, :], Et.t[:, 512:1024], start=(kb == 0), stop=(kb == NT - 1)),
                     reads=[Vh[b], Et], writes=[psO])
                j = kb % 4
                S.op("pe", lambda e, Et=Et, kb=kb, j=j: e.matmul(
                    psZ.t[32 * j:32 * (j + 1), :], K.onesB.t[:, 0:32], Et.t[:, 0:512],
                    start=(kb < 4), stop=(kb >= NT - 4), tile_position=(0, 32 * j)),
                    reads=[K.onesB, Et], writes=[psZ])
                if kb == 0:
                    S.op("dve", lambda e, Et=Et: e.tensor_copy(ac2.t[:, :], Et.t[:, 512:1024]), reads=[Et], writes=[ac2])
                else:
                    S.op("dve", lambda e, Et=Et: e.tensor_tensor(ac2.t[:, :], ac2.t[:, :], Et.t[:, 512:1024], ALU.add), reads=[Et, ac2], writes=[ac2])
            it += NT
            S.op("dve", lambda e: e.tensor_copy(zs.t[:, :], psZ.t[:, :]), reads=[psZ], writes=[zs])
            S.op("pe", lambda e: e.matmul(psB.t[:, :], sel1.t[:, :], zs.t[:, :], start=True, stop=True), reads=[sel1, zs], writes=[psB])
            S.op("pe", lambda e: e.matmul(psZ.t[:, :], K.onesF.t[:, :], ac2.t[:, :], start=True, stop=True), reads=[K.onesF, ac2, zs], writes=[psZ])
            S.op("dve", lambda e: e.reciprocal(rr.t[:, 0:512], psB.t[:, :]), reads=[psB], writes=[rr])
            S.op("dve", lambda e: e.reciprocal(rr.t[:, 512:1024], psZ.t[:, :]), reads=[psZ], writes=[rr])
            S.op("dve", lambda e: e.tensor_tensor(o1.t[:, :], psO.t[:, 0:512], rr.t[:, 0:512], ALU.mult), reads=[psO, rr], writes=[o1])
            S.op("dve", lambda e: e.tensor_tensor(o2.t[:, :], psO.t[:, 512:1024], rr.t[:, 512:1024], ALU.mult), reads=[psO, rr], writes=[o2])
            dd = dao[nq % 2]

            def part_b():
                S.op("dve", lambda e: e.scalar_tensor_tensor(oo.t[:, :], o2.t[:, :], v.t[:, 25:26], o1.t[:, :], ALU.mult, ALU.add), reads=[o1, o2, v], writes=[oo])
                S.op("act", lambda e: e.activation(sq.t[:, :], oo.t[:, :], AF.Square), reads=[oo], writes=[sq])
                S.op("pe", lambda e: e.matmul(psB.t[:, :], K.onesF.t[:, :], sq.t[:, :], start=True, stop=True), reads=[K.onesF, sq], writes=[psB])
                S.op("act", lambda e: e.activation(lnv.t[:, :], psB.t[:, :], AF.Ln, bias=EPS, scale=1.0 / 128), reads=[psB], writes=[lnv])
                S.op("act", lambda e: e.activation(rstd.t[:, :], lnv.t[:, :], AF.Exp, scale=-0.5), reads=[lnv], writes=[rstd])
                S.op("dve", lambda e: e.scalar_tensor_tensor(dd.t[:, :], oo.t[:, :], v.t[:, 26:27], rstd.t[:, :], ALU.mult, ALU.mult), reads=[oo, rstd, v], writes=[dd])
                S.dma("sp", K.MIXT[h, :, qs], dd.t[:, :], reads=[dd])

            pendB.append(part_b)
            nq += 1
            return it, nq

        load_head(0)
        it = 0
        nq = 0
        for h in range(4):
            b = h % 2
            if h + 1 < 4:
                load_head(h + 1)
            for qb in range(NQB):
                it, nq = attn_block(h, b, qb, it, nq)
        while pendB:
            pendB.pop(0)()
        S.flush()
    S.mes = S.es


def phase4(K):
    S = K.S
    sm, v, mf, bc = K.sm, K.vec, K.modfm, K.bc
    with ExitStack() as mes:
        S.mes = mes
        Wo = S.sb("Wo", [128, 8, D], BF16)
        W1 = S.sb("W1", [128, 8, DFF], BF16)
        W2 = S.sb("W2", [128, 32, D], BF16)
        S.dma("pool", Wo.t[:, 0:4, :], K.w_out[0:512, :].rearrange("(k p) f -> p k f", p=128), writes=[Wo])
        with ExitStack() as sub:
            S.mes = sub
            wof = S.sb("wof", [128, 4, D], F32)
            S.dma("sp", wof.t[:, :, :], K.w_out[512:1024, :].rearrange("(k p) f -> p k f", p=128), writes=[wof])
            for hh in range(4):
                S.op("dve", lambda e, hh=hh: e.tensor_scalar(Wo.t[:, 4 + hh, :], wof.t[:, hh, :], sm.t[:, SM_MLG + hh:SM_MLG + hh + 1], None, ALU.mult),
                     reads=[wof, sm], writes=[Wo])
            S.flush()
        S.mes = mes
        for kc in range(8):
            S.dma("pool", W1.t[:, kc, :], K.w_fc1[kc * 128:(kc + 1) * 128, :], writes=[W1])
        for g in range(4):
            S.dma("pool", W2.t[:, 8 * g:8 * (g + 1), :], K.w_fc2[1024 * g:1024 * (g + 1), :].rearrange("(k p) f -> p k f", p=128), writes=[W2])
        xt2 = [S.sb(f"x2_{i}", [128, 2, D], F32) for i in range(2)]
        mix = [S.sb(f"mix{i}", [128, 8, 256], BF16) for i in range(2)]
        xn = S.sb("xn", [128, 2, D], BF16)
        xm2 = S.sb("xm2", [128, 8, 256], BF16)
        tmp = [S.sb(f"tmp{i}", [128, 512], F32) for i in range(2)]
        r1 = [S.sb(f"r1{i}", [128, 256], F32) for i in range(3)]
        h1 = [S.sb(f"h1{i}", [128, 256], BF16) for i in range(4)]
        st4 = [S.sb(f"st4{i}", [128, 16], F32) for i in range(2)]
        junk = S.sb("junk4", [128, D], BF16)
        psF = [[S.ps(f"psF{i}{hf}", [128, 512], F32) for hf in range(2)] for i in range(2)]
        pst = S.ps("pst4", [128, 8, 128], BF16)
        ps1 = [S.ps(f"ps1{i}", [128, 256], F32) for i in range(3)]
        nblk = TOWN // 256
        ntmp = 0
        nf = 0

        def load_blk(bi):
            p = bi % 2
            t0 = bi * 256
            S.dma("sp", xt2[p].t[:, :, :], K.x[t0:t0 + 256, :].rearrange("(i p) d -> p i d", p=128), writes=[xt2[p]])
            S.dma("sp", mix[p].t[:, :, :], K.MIXT[:, :, t0:t0 + 256].rearrange("c p t -> p c t"), writes=[mix[p]])

        cnt = {'ntmp': 0, 'nf': 0}

        def do_block(bi, p, t0):
            ntmp = cnt['ntmp']
            nf = cnt['nf']
            X = xt2[p]
            sst = st4[p]
            for i in range(2):
                for hf in range(2):
                    ps = psF[i][hf]
                    hs_ = slice(hf * 512, (hf + 1) * 512)
                    for c in range(8):
                        S.op("pe", lambda e, ps=ps, c=c, i=i, hs_=hs_: e.matmul(ps.t[:, :], mix[p].t[:, c, i * 128:(i + 1) * 128], Wo.t[:, c, hs_],
                                                                           start=(c == 0), stop=(c == 7)),
                             reads=[mix[p], Wo], writes=[ps], inc=(c == 7))
                    tm = tmp[ntmp % 2]; ntmp += 1
                    S.op("dve", lambda e, ps=ps, tm=tm, hs_=hs_: e.tensor_tensor(tm.t[:, :], ps.t[:, :], bc.t[:, 0, hs_], ALU.mult), reads=[ps, bc], writes=[tm])
                    S.op("pool", lambda e, tm=tm, i=i, hs_=hs_: e.tensor_tensor(X.t[:, i, hs_], tm.t[:, :], X.t[:, i, hs_], ALU.add), reads=[tm, X], writes=[X])
            for i in range(2):
                S.op("act", lambda e, i=i: e.activation(junk.t[:, :], X.t[:, i, :], AF.Square, accum_out=sst.t[:, i:i + 1]), reads=[X], writes=[junk, sst])
            S.op("act", lambda e: e.activation(sst.t[:, 2:4], sst.t[:, 0:2], AF.Ln, bias=EPS, scale=1.0 / D), reads=[sst], writes=[sst])
            S.op("act", lambda e: e.activation(sst.t[:, 4:6], sst.t[:, 2:4], AF.Exp, scale=-0.5), reads=[sst], writes=[sst])
            for i in range(2):
                S.op("dve", lambda e, i=i: e.tensor_scalar(xn.t[:, i, :], X.t[:, i, :], sst.t[:, 4 + i:5 + i], None, ALU.mult), reads=[X, sst], writes=[xn])
            for i in range(2):
                for kc in range(8):
                    S.op("pe", lambda e, i=i, kc=kc: e.transpose(pst.t[:, kc, :], xn.t[:, i, kc * 128:(kc + 1) * 128], K.identB.t[:, :]),
                         reads=[xn, K.identB], writes=[pst], inc=(kc == 7))
                for kc in range(8):
                    if kc % 2 == 0:
                        S.op("act", lambda e, i=i, kc=kc: e.activation(xm2.t[:, kc, i * 128:(i + 1) * 128], pst.t[:, kc, :], AF.Identity,
                                                                     scale=v.t[:, 16 + kc:17 + kc], bias=mf.t[:, 24 + kc, 0:1]),
                             reads=[pst, v, mf], writes=[xm2])
                    else:
                        S.op("dve", lambda e, i=i, kc=kc: e.tensor_scalar(xm2.t[:, kc, i * 128:(i + 1) * 128], pst.t[:, kc, :],
                                                                        v.t[:, 16 + kc:17 + kc], mf.t[:, 24 + kc, 0:1], ALU.mult, ALU.add),
                             reads=[pst, v, mf], writes=[xm2])
            def ffn1(f, nfi):
                p1 = ps1[nfi % 3]
                rt = r1[nfi % 3]
                ht = h1[nfi % 4]
                for kc in range(8):
                    S.op("pe", lambda e, kc=kc: e.matmul(p1.t[:, :], W1.t[:, kc, f * 128:(f + 1) * 128], xm2.t[:, kc, :], start=(kc == 0), stop=(kc == 7)),
                         reads=[W1, xm2], writes=[p1], inc=(kc == 7))
                S.op("act", lambda e: e.activation(rt.t[:, :], p1.t[:, :], AF.Relu), reads=[p1], writes=[rt])
                S.op("dve", lambda e: e.tensor_tensor(ht.t[:, :], rt.t[:, :], rt.t[:, :], ALU.mult), reads=[rt], writes=[ht])
                return ht

            def ffn2(f, ht):
                for i in range(2):
                    for hf in range(2):
                        S.op("pe", lambda e, i=i, hf=hf: e.matmul(psF[i][hf].t[:, :], ht.t[:, i * 128:(i + 1) * 128], W2.t[:, f, hf * 512:(hf + 1) * 512],
                                                                  start=(f == 0), stop=(f == 31)),
                             reads=[ht, W2], writes=[psF[i][hf]], inc=(i == 1 and hf == 1))

            hts = {}
            hts[0] = ffn1(0, nf)
            hts[1] = ffn1(1, nf + 1)
            for f in range(32):
                if f + 2 < 32:
                    hts[f + 2] = ffn1(f + 2, nf + f + 2)
                ffn2(f, hts.pop(f))
            nf += 32
            for i in range(2):
                for hf in range(2):
                    hs_ = slice(hf * 512, (hf + 1) * 512)
                    tm = tmp[ntmp % 2]; ntmp += 1
                    S.op("dve", lambda e, i=i, hf=hf, tm=tm, hs_=hs_: e.tensor_tensor(tm.t[:, :], psF[i][hf].t[:, :], bc.t[:, 1, hs_], ALU.mult),
                         reads=[psF[i][hf], bc], writes=[tm])
                    S.op("pool", lambda e, tm=tm, i=i, hs_=hs_: e.tensor_tensor(X.t[:, i, hs_], tm.t[:, :], X.t[:, i, hs_], ALU.add), reads=[tm, X], writes=[X])
            for i in range(2):
                S.op("act", lambda e, i=i: e.activation(junk.t[:, :], X.t[:, i, :], AF.Square, accum_out=sst.t[:, 8 + i:9 + i]), reads=[X], writes=[junk, sst])
            S.op("act", lambda e: e.activation(sst.t[:, 10:12], sst.t[:, 8:10], AF.Ln, bias=EPS, scale=1.0 / D), reads=[sst], writes=[sst])
            S.op("act", lambda e: e.activation(sst.t[:, 12:14], sst.t[:, 10:12], AF.Exp, scale=-0.5), reads=[sst], writes=[sst])
            for i in range(2):
                S.op("dve", lambda e, i=i: e.scalar_tensor_tensor(X.t[:, i, :], X.t[:, i, :], sst.t[:, 12 + i:13 + i], bc.t[:, 2, :], ALU.mult, ALU.mult),
                     reads=[X, sst, bc], writes=[X])
            S.dma("sp", K.out[t0:t0 + 256, :].rearrange("(i p) d -> p i d", p=128), X.t[:, :, :], reads=[X], out=True)
            cnt['ntmp'] = ntmp
            cnt['nf'] = nf

        load_blk(0)
        for bi in range(nblk):
            if bi + 1 < nblk:
                load_blk(bi + 1)
            do_block(bi, bi % 2, bi * 256)
        S.flush()
    S.mes = S.es


def _rope_tables(half):
    g = np.arange(T, dtype=np.int64)
    if half == 1:
        g = g[::-1]
    row = (g // 64).astype(np.float32)
    col = (g % 64).astype(np.float32)
    inv = (np.float32(10000.0) ** (-np.arange(0, 32, 2, dtype=np.float32) / np.float32(32))).astype(np.float32)
    ar = row[:, None] * inv
    ac = col[:, None] * inv
    ang = np.concatenate([ar, ar, ac, ac], axis=-1).astype(np.float32)
    cos = np.cos(ang).astype(np.float32)
    sin = np.sin(ang).astype(np.float32)
    sign = np.where((np.arange(64) % 32) < 16, -1.0, 1.0).astype(np.float32)
    sin = sin * sign[None, :]
    cosT = np.ascontiguousarray(np.concatenate([cos.T, cos.T], axis=0))
    sinT = np.ascontiguousarray(np.concatenate([sin.T, sin.T], axis=0))
    return cosT, sinT


def _w_cols(half):
    DQ0, DK0, DV0, MQ0, MK0, MV0, MO0, MG0 = 0, 512, 1024, 1536, 1792, 2048, 2560, 3072
    partner = [d + 16 if (d % 32) < 16 else d - 16 for d in range(64)]
    cols = []
    for h in range(4):
        for base in (DQ0, DK0):
            cols += [base + h * 128 + m * 64 + d for m in range(2) for d in range(64)]
            cols += [base + h * 128 + m * 64 + partner[d] for m in range(2) for d in range(64)]
    cols += list(range(MQ0, MQ0 + 256)) + list(range(MK0, MK0 + 256))
    cols += list(range(DV0, DV0 + 512)) + list(range(MV0, MV0 + 512)) + list(range(MO0, MO0 + 512))
    cols += list(range(MK0, MK0 + 256))
    gorder = list(range(16)) if half == 0 else (list(range(8, 16)) + list(range(0, 8)))
    cols += [MG0 + g for g in gorder]
    assert len(cols) == WCOLS
    return np.asarray(cols), gorder


def host_layout(inp):
    f32 = np.float32
    x = np.asarray(inp["x"], f32)
    ctx = np.asarray(inp["ctx"], f32)
    c = np.asarray(inp["c"], f32)
    c_ctx = np.asarray(inp["c_ctx"], f32)
    w_in = np.asarray(inp["w_in"], f32)[0]
    w_ada = np.ascontiguousarray(np.asarray(inp["w_ada"], f32)[0])
    b_ada = np.asarray(inp["b_ada"], f32)[0]
    w_out = np.ascontiguousarray(np.asarray(inp["w_out"], f32)[0])
    w_fc1 = np.ascontiguousarray(np.asarray(inp["w_fc1"], f32)[0])
    w_fc2 = np.ascontiguousarray(np.asarray(inp["w_fc2"], f32)[0])
    tri = np.triu(np.ones((128, 128), f32))
    per_half = []
    for half in range(2):
        cols, gorder = _w_cols(half)
        w_loc = np.ascontiguousarray(w_in[:, cols])
        cosT, sinT = _rope_tables(half)
        per_half.append((w_loc, cosT, sinT, gorder))
    bc_in = np.empty((128, 3, D), f32)
    bc_in[:, 0, :] = b_ada[2 * D:3 * D][None, :]
    bc_in[:, 1, :] = b_ada[5 * D:6 * D][None, :]
    bc_in[:, 2, :] = np.asarray(inp["final_g"], f32)[None, :]
    maps = []
    for core in range(8):
        b, half = core // 2, core % 2
        w_loc, cosT, sinT, gorder = per_half[half]
        smalls = np.zeros((128, NS), f32)
        ct = np.stack([c[b].reshape(8, 128).T, c_ctx.reshape(8, 128).T], axis=-1)
        smalls[:, SM_CT:SM_CT + 16] = ct.reshape(128, 16)
        smalls[:, SM_BADA:SM_BADA + 48] = b_ada.reshape(48, 128).T
        smalls[:, SM_NG:SM_NG + 8] = np.asarray(inp["norm1_g"], f32)[0].reshape(8, 128).T
        smalls[:, SM_NG + 8:SM_NG + 16] = np.asarray(inp["norm2_g"], f32)[0].reshape(8, 128).T
        for i, nm in enumerate(("lam_q1", "lam_k1", "lam_q2", "lam_k2")):
            smalls[:, SM_LAM + 64 * i:SM_LAM + 64 * (i + 1)] = np.asarray(inp[nm], f32)[0][None, :]
        smalls[:, SM_SUB] = np.asarray(inp["subln_g"], f32)[0]
        smalls[:, SM_MLG:SM_MLG + 4] = np.asarray(inp["mlstm_norm_g"], f32)[0].reshape(4, 128).T
        smalls[:, SM_BG:SM_BG + 16] = np.asarray(inp["b_gate"], f32)[0][gorder][None, :]
        smalls[:, SM_TRIF:SM_TRIF + 128] = tri
        smalls[:, SM_TRIB:SM_TRIB + 128] = tri.T
        smalls[:, SM_ID:SM_ID + 128] = np.eye(128, dtype=f32)
        xl = x[b] if half == 0 else x[b][::-1]
        cl = ctx[b] if half == 0 else ctx[b][::-1]
        maps.append({
            "x_loc": np.ascontiguousarray(xl), "ctx_loc": np.ascontiguousarray(cl),
            "w_loc": w_loc, "w_ada": w_ada, "w_out": w_out, "w_fc1": w_fc1, "w_fc2": w_fc2,
            "cosT": cosT, "sinT": sinT, "smalls": smalls, "bc_in": bc_in,
        })
    return maps


_NC_CACHE = {}


def kernel(**inputs):
    maps = host_layout(inputs)
    if "nc" not in _NC_CACHE:
        _NC_CACHE["nc"] = build_program()
    nc = _NC_CACHE["nc"]
    res = run_bass_kernel_spmd(nc, maps, core_ids=list(range(8)))
    out = np.empty((4, T, D), np.float32)
    for core in range(8):
        b, half = core // 2, core % 2
        o = np.asarray(res.results[core]["out"])
        if half == 0:
            out[b, 0:TOWN] = o
        else:
            out[b, TOWN:T] = o[::-1]
    return out
```

```python
import numpy as np
import ml_dtypes
import concourse.bass as bass
import concourse.mybir as mybir
from concourse.bass_utils import run_bass_kernel_spmd
from contextlib import ExitStack

F32 = mybir.dt.float32
BF16 = mybir.dt.bfloat16
AF = mybir.ActivationFunctionType
ALU = mybir.AluOpType
AX = mybir.AxisListType


class Tok:
    def __init__(self, name="tok"):
        self.name = name
        self.writer = None
        self.readers = []


class TT(Tok):
    def __init__(self, t, name):
        Tok.__init__(self, name)
        self.t = t


class Sched:
    ENG = ("pe", "act", "dve", "pool", "sp")
    LIMIT = 20000

    def __init__(self, nc, es, ndma=24):
        self.nc = nc
        self.es = es
        self.mes = es
        self.ops = {e: [] for e in self.ENG}
        self.cnt = {e: 0 for e in self.ENG}
        self.epoch = {e: 0 for e in self.ENG}
        self.seen = {e: {} for e in self.ENG}
        self.sems = {}
        self.pend = {e: ([], []) for e in self.ENG}
        self.ndma = ndma
        self.dma_issued = [0] * ndma
        self.dma_rr = 0
        self.dma_rr_sw = 0
        self.out_tokens = []
        self.nblk = 0
        self.uid = 0
        for i in range(ndma):
            self._sem(("dma", i))
        for e in self.ENG:
            self._sem((e, 0))

    def _sem(self, key):
        if key not in self.sems:
            nm = "s_" + "_".join(str(k) for k in key)
            self.sems[key] = self.es.enter_context(self.nc.semaphore(nm))
        return self.sems[key]

    def sb(self, name, shape, dtype):
        self.uid += 1
        t = self.mes.enter_context(self.nc.sbuf_tensor(f"{name}_{self.uid}", list(shape), dtype))
        return TT(t, name)

    def ps(self, name, shape, dtype):
        self.uid += 1
        t = self.mes.enter_context(self.nc.psum_tensor(f"{name}_{self.uid}", list(shape), dtype))
        return TT(t, name)

    def _wait(self, eng, tok):
        if tok is None:
            return
        key, val = tok
        if self.seen[eng].get(key, 0) >= val:
            return
        if eng == "pe" and key[0] == "pe":
            return
        self.seen[eng][key] = val
        sem = self.sems[key]
        self.ops[eng].append(lambda e, sem=sem, val=val: e.wait_ge(sem, val))

    def _deps(self, eng, reads, writes):
        for b in reads:
            self._wait(eng, b.writer)
        for b in writes:
            self._wait(eng, b.writer)
            for r in b.readers:
                self._wait(eng, r)

    def op(self, eng, fn, reads=(), writes=(), inc=True):
        self._deps(eng, reads, writes)
        pr, pw = self.pend[eng]
        pr.extend(reads)
        pw.extend(writes)
        if not inc:
            self.ops[eng].append(lambda e, fn=fn: fn(e))
            return None
        if self.cnt[eng] >= self.LIMIT:
            self.epoch[eng] += 1
            self.cnt[eng] = 0
            self._sem((eng, self.epoch[eng]))
        key = (eng, self.epoch[eng])
        sem = self.sems[key]
        self.cnt[eng] += 1
        tok = (key, self.cnt[eng])
        self.ops[eng].append(lambda e, fn=fn, sem=sem: fn(e).then_inc(sem, 1))
        for b in pw:
            b.writer = tok
            b.readers = []
        for b in pr:
            if b.writer is not tok:
                b.readers.append(tok)
        self.pend[eng] = ([], [])
        return tok

    def dma(self, eng, out_ap, in_ap, reads=(), writes=(), out=False, **kw):
        self._deps(eng, reads, writes)
        if eng == "pool":
            i = self.ndma - 8 + self.dma_rr_sw
            self.dma_rr_sw = (self.dma_rr_sw + 1) % 8
        else:
            i = self.dma_rr
            self.dma_rr = (self.dma_rr + 1) % (self.ndma - 8)
        key = ("dma", i)
        if self.dma_issued[i] > 0:
            self._wait(eng, (key, 16 * self.dma_issued[i]))
        sem = self.sems[key]
        self.dma_issued[i] += 1
        tok = (key, 16 * self.dma_issued[i])
        self.ops[eng].append(
            lambda e, sem=sem, o=out_ap, a=in_ap, kw=kw: e.dma_start(out=o, in_=a, **kw).then_inc(sem, 16))
        for b in writes:
            b.writer = tok
            b.readers = []
        for b in reads:
            b.readers.append(tok)
        if out:
            self.out_tokens.append(tok)
        return tok

    def flush(self, final=False):
        if final:
            for tok in self.out_tokens:
                self._wait("sp", tok)
            for e in ("pe", "act", "dve", "pool"):
                if self.cnt[e] > 0:
                    self._wait("sp", ((e, self.epoch[e]), self.cnt[e]))
        else:
            self.barrier()
        ops = self.ops
        self.ops = {e: [] for e in self.ENG}
        self.nblk += 1
        with self.nc.Block() as block:
            @block.tensor
            def _(pe):
                for f in ops["pe"]:
                    f(pe)

            @block.scalar
            def _(act):
                for f in ops["act"]:
                    f(act)

            @block.vector
            def _(dve):
                for f in ops["dve"]:
                    f(dve)

            @block.gpsimd
            def _(pool):
                for f in ops["pool"]:
                    f(pool)

            @block.sync
            def _(sp):
                for f in ops["sp"]:
                    f(sp)

    def barrier(self):
        for e in ("pe", "act", "dve", "pool"):
            if self.cnt[e] > 0:
                self._wait("sp", ((e, self.epoch[e]), self.cnt[e]))
        for i in range(self.ndma):
            if self.dma_issued[i] > 0:
                self._wait("sp", (("dma", i), 16 * self.dma_issued[i]))
        if self.cnt["sp"] >= self.LIMIT:
            self.epoch["sp"] += 1
            self.cnt["sp"] = 0
            self._sem(("sp", self.epoch["sp"]))
        key = ("sp", self.epoch["sp"])
        sem = self.sems[key]
        self.cnt["sp"] += 1
        tok = (key, self.cnt["sp"])
        self.ops["sp"].append(lambda e, sem=sem: e.sem_inc(sem, 1))
        for e in ("pe", "act", "dve", "pool"):
            self._wait(e, tok)

    def finish(self):
        self.flush(final=True)


D = 1024
T = 8192
TOWN = 4096
NCTX = 256
NKEY = T + NCTX
NT = NKEY // 128
DFF = 4096
EPS = 1e-6
NFM = 20
TM0 = NFM * 128
WCOLS = TM0 + 1808
C_DV, C_MV, C_MO, C_MK, C_G = TM0, TM0 + 512, TM0 + 1024, TM0 + 1536, TM0 + 1792
SM_CT = 0
SM_BADA = 16
SM_NG = 64
SM_LAM = 80
SM_SUB = 336
SM_MLG = 337
SM_BG = 341
SM_TRIF = 357
SM_TRIB = 485
SM_ID = 613
NS = 741


class Ctx:
    pass


def build_program(debug=None, phases=(0, 1, 2, 3, 4)):
    nc = bass.Bass("TRN2", target_bir_lowering=False)
    K = Ctx()
    K.nc = nc
    K.debug = debug or ()

    def din(name, shape, dt=F32):
        return nc.dram_tensor(name, list(shape), dt, kind="ExternalInput").ap()

    def dscr(name, shape, dt):
        kind = "ExternalOutput" if name in K.debug else "Internal"
        return nc.dram_tensor(name, list(shape), dt, kind=kind).ap()

    K.x = din("x_loc", [T, D])
    K.ctx = din("ctx_loc", [NCTX, D])
    K.w_in = din("w_loc", [D, WCOLS])
    K.w_ada = din("w_ada", [D, 6 * D])
    K.w_out = din("w_out", [D, D])
    K.w_fc1 = din("w_fc1", [D, DFF])
    K.w_fc2 = din("w_fc2", [DFF, D])
    K.cosT = din("cosT", [128, T])
    K.sinT = din("sinT", [128, T])
    K.smalls = din("smalls", [128, NS])
    K.bc_in = din("bc_in", [128, 3, D])
    K.out = nc.dram_tensor("out", [TOWN, D], F32, kind="ExternalOutput").ap()
    K.QT = dscr("QT", [4, 128, TOWN], BF16)
    K.KT = dscr("KT", [4, 128, NKEY], BF16)
    K.V = dscr("V", [NKEY, 512], BF16)
    K.MV = dscr("MV", [NKEY, 512], BF16)
    K.MK = dscr("MK", [NKEY, 256], BF16)
    K.G = dscr("G", [NKEY, 16], F32)
    K.MQT = dscr("MQT", [2, 128, TOWN], BF16)
    K.MKT = dscr("MKT", [2, 128, TOWN], BF16)
    K.MO = dscr("MO", [TOWN, 512], BF16)
    K.MIXT = dscr("MIXT", [8, 128, TOWN], BF16)
    K.DBG = dscr("DBG", [128, 2048], F32)

    with ExitStack() as es:
        S = Sched(nc, es)
        K.S = S
        K.tok = {n: Tok(n) for n in ("QT", "KT", "V", "MV", "MK", "G", "MQT", "MKT", "MO", "MIXT")}
        phase0(K)
        if 1 in phases:
            phase1(K)
        if 2 in phases:
            phase2(K)
        if 3 in phases:
            phase3(K)
        if 4 in phases:
            phase4(K)
        S.finish()
    return nc


def phase0(K):
    S = K.S
    sm = K.sm = S.sb("sm", [128, NS], F32)
    S.dma("sp", sm.t[:, :], K.smalls, writes=[sm])
    K.identB = S.sb("identB", [128, 128], BF16)
    K.onesF = S.sb("onesF", [128, 128], F32)
    K.onesB = S.sb("onesB", [128, 128], BF16)
    K.modfm = S.sb("modfm", [128, 48, 2], F32)
    K.vec = S.sb("vec", [128, 64], F32)
    K.bc = S.sb("bc", [128, 3, D], F32)
    S.dma("sp", K.bc.t[:, :, :], K.bc_in, writes=[K.bc])
    S.op("dve", lambda e: e.tensor_copy(K.identB.t[:, :], sm.t[:, SM_ID:SM_ID + 128]), reads=[sm], writes=[K.identB])
    S.op("dve", lambda e: e.memset(K.onesF.t[:, :], 1.0), writes=[K.onesF])
    S.op("dve", lambda e: e.memset(K.onesB.t[:, :], 1.0), writes=[K.onesB])
    with ExitStack() as mes:
        S.mes = mes
        sc = S.sb("sc", [128, 16], F32)
        screp = S.sb("screp", [128, 8, 128], F32)
        wa = [S.sb("wa0", [128, 8, 1024], F32), S.sb("wa1", [128, 8, 1024], F32)]
        psm = S.ps("psm", [128, 96], F32)
        psg = [S.ps("psg0", [128, 512], F32), S.ps("psg1", [128, 512], F32)]
        S.op("act", lambda e: e.activation(sc.t[:, :], sm.t[:, SM_CT:SM_CT + 16], AF.Silu), reads=[sm], writes=[sc])
        for kc in range(8):
            S.op("dve", lambda e, kc=kc: e.tensor_scalar(screp.t[:, kc, :], K.onesF.t[:, :], sc.t[:, 2 * kc:2 * kc + 1], None, ALU.mult),
                 reads=[K.onesF, sc], writes=[screp])
        for piece in range(6):
            w = wa[piece % 2]
            S.dma("sp", w.t[:, :, :], K.w_ada[:, piece * D:(piece + 1) * D].rearrange("(k p) f -> p k f", p=128), writes=[w])
            for fcl in range(8):
                fc = piece * 8 + fcl
                for kc in range(8):
                    S.op("pe", lambda e, w=w, fc=fc, fcl=fcl, kc=kc: e.matmul(
                        psm.t[:, 2 * fc:2 * fc + 2], w.t[:, kc, fcl * 128:(fcl + 1) * 128],
                        sc.t[:, 2 * kc:2 * kc + 2], start=(kc == 0), stop=(kc == 7)),
                        reads=[w, sc], writes=[psm], inc=(kc == 7))
            if piece in (2, 5):
                gi = 0 if piece == 2 else 1
                for half in range(2):
                    pg = psg[half]
                    for kc in range(8):
                        S.op("pe", lambda e, w=w, pg=pg, half=half, kc=kc: e.matmul(
                            pg.t[:, :], screp.t[:, kc, :], w.t[:, kc, half * 512:(half + 1) * 512],
                            start=(kc == 0), stop=(kc == 7)), reads=[w, screp], writes=[pg], inc=(kc == 7))
                    S.op("dve", lambda e, pg=pg, gi=gi, half=half: e.tensor_tensor(
                        K.bc.t[:, gi, half * 512:(half + 1) * 512], pg.t[:, :],
                        K.bc.t[:, gi, half * 512:(half + 1) * 512], ALU.add), reads=[pg, K.bc], writes=[K.bc])
        pm3 = psm.t[:, :].rearrange("p (f j) -> p f j", j=2)
        for j in range(2):
            S.op("dve", lambda e, j=j: e.tensor_tensor(K.modfm.t[:, :, j], pm3[:, :, j], sm.t[:, SM_BADA:SM_BADA + 48], ALU.add),
                 reads=[psm, sm], writes=[K.modfm])
        v = K.vec
        mf = K.modfm
        n1g = sm.t[:, SM_NG:SM_NG + 8]
        n2g = sm.t[:, SM_NG + 8:SM_NG + 16]
        S.op("dve", lambda e: e.scalar_tensor_tensor(v.t[:, 0:8], mf.t[:, 8:16, 0], 1.0, n1g, ALU.add, ALU.mult), reads=[mf, sm], writes=[v])
        S.op("dve", lambda e: e.scalar_tensor_tensor(v.t[:, 8:16], mf.t[:, 8:16, 1], 1.0, n1g, ALU.add, ALU.mult), reads=[mf, sm], writes=[v])
        S.op("dve", lambda e: e.scalar_tensor_tensor(v.t[:, 16:24], mf.t[:, 32:40, 0], 1.0, n2g, ALU.add, ALU.mult), reads=[mf, sm], writes=[v])
        lt = S.sb("lt", [128, 128], F32)
        S.op("dve", lambda e: e.tensor_tensor(lt.t[:, 0:64], sm.t[:, SM_LAM:SM_LAM + 64], sm.t[:, SM_LAM + 64:SM_LAM + 128], ALU.mult), reads=[sm], writes=[lt])
        S.op("dve", lambda e: e.tensor_tensor(lt.t[:, 64:128], sm.t[:, SM_LAM + 128:SM_LAM + 192], sm.t[:, SM_LAM + 192:SM_LAM + 256], ALU.mult), reads=[sm], writes=[lt])
        S.op("dve", lambda e: e.reduce_sum(v.t[:, 27:28], lt.t[:, 0:64], AX.X), reads=[lt], writes=[v])
        S.op("dve", lambda e: e.reduce_sum(v.t[:, 28:29], lt.t[:, 64:128], AX.X), reads=[lt], writes=[v])
        S.op("act", lambda e: e.activation(v.t[:, 29:31], v.t[:, 27:29], AF.Exp), reads=[v], writes=[v])
        S.op("dve", lambda e: e.tensor_tensor(v.t[:, 24:25], v.t[:, 29:30], v.t[:, 30:31], ALU.subtract), reads=[v], writes=[v])
        S.op("dve", lambda e: e.tensor_scalar(v.t[:, 24:25], v.t[:, 24:25], 0.2, None, ALU.add), reads=[v], writes=[v])
        S.op("dve", lambda e: e.tensor_scalar(v.t[:, 25:26], v.t[:, 24:25], -1.0, None, ALU.mult), reads=[v], writes=[v])
        S.op("dve", lambda e: e.tensor_scalar(v.t[:, 26:27], sm.t[:, SM_SUB:SM_SUB + 1], 0.8, None, ALU.mult), reads=[sm], writes=[v])
        if "DBG" in K.debug:
            dbg = S.sb("dbg", [128, 2048], F32)
            S.op("dve", lambda e: e.memset(dbg.t[:, :], 0.0), writes=[dbg])
            S.op("dve", lambda e: e.tensor_copy(dbg.t[:, 0:96], mf.t[:, :, :].rearrange("p f j -> p (f j)")), reads=[mf], writes=[dbg])
            S.op("dve", lambda e: e.tensor_copy(dbg.t[:, 96:160], v.t[:, :]), reads=[v], writes=[dbg])
            S.op("dve", lambda e: e.tensor_copy(dbg.t[:, 1024:2048], K.bc.t[:, 0, :]), reads=[K.bc], writes=[dbg])
            S.dma("sp", K.DBG, dbg.t[:, :], reads=[dbg], out=True)
        S.flush()
    S.mes = S.es


def phase1(K):
    S = K.S
    sm, v, mf = K.sm, K.vec, K.modfm
    with ExitStack() as mes:
        S.mes = mes
        Wb = S.sb("Wb", [128, 8, WCOLS], BF16)
        for kc in range(8):
            S.dma("pool", Wb.t[:, kc, :], K.w_in[kc * 128:(kc + 1) * 128, :], writes=[Wb])
        NB = 2
        xt = [S.sb("xt", [128, 4, D], F32)] * NB
        xh = [S.sb("xh", [128, 4, D], BF16)] * NB
        xm = [S.sb(f"xm{i}", [128, 8, 512], BF16) for i in range(NB)]
        st = [S.sb(f"st{i}", [128, 16], F32) for i in range(NB)]
        junk = S.sb("junk", [128, D], BF16)
        cs = [S.sb(f"cs{i}", [128, 2, 512], F32) for i in range(NB)]
        qk = [S.sb("qk", [128, 8, 512], BF16)] * NB
        t1 = [S.sb(f"t1{i}", [128, 512], F32) for i in range(2)]
        t2 = [S.sb(f"t2{i}", [128, 512], F32) for i in range(2)]
        fmo = [S.sb("fmo", [128, 4, 512], BF16)] * NB
        tmo = [S.sb("tmo", [128, 4, 1792], BF16)] * NB
        gto = [S.sb("gto", [128, 4, 16], F32)] * NB
        pst = [S.ps(f"pst{i}", [128, 2, 512], BF16) for i in range(2)]
        psf = [S.ps(f"psf{i}", [128, 512], F32) for i in range(4)]
        pso = [S.ps(f"pso{i}", [128, 512], F32) for i in range(2)]
        nps = 0
        npo = 0
        blocks = [("own", i) for i in range(8)] + [("other", i) for i in range(8)] + [("ctx", 0)]
        def blk_geom(bi):
            kind, ib = blocks[bi]
            nt = 2 if kind == "ctx" else 4
            if kind == "ctx":
                tok0 = T
            else:
                tok0 = ib * 512 + (TOWN if kind == "other" else 0)
            return kind, nt, tok0

        def prep(bi):
            kind, nt, tok0 = blk_geom(bi)
            p = bi % NB
            if kind == "ctx":
                src = K.ctx.rearrange("(i p) d -> p i d", p=128)
            else:
                src = K.x[tok0:tok0 + 512, :].rearrange("(i p) d -> p i d", p=128)
            S.dma("sp", xt[p].t[:, 0:nt, :], src, writes=[xt[p]])
            if kind != "ctx":
                S.dma("sp", cs[p].t[:, 0, :], K.cosT[:, tok0:tok0 + 512], writes=[cs[p]])
                S.dma("sp", cs[p].t[:, 1, :], K.sinT[:, tok0:tok0 + 512], writes=[cs[p]])
            for i in range(nt):
                S.op("act", lambda e, p=p, i=i: e.activation(junk.t[:, :], xt[p].t[:, i, :], AF.Square, accum_out=st[p].t[:, i:i + 1]),
                     reads=[xt[p]], writes=[junk, st[p]])
            S.op("act", lambda e, p=p, nt=nt: e.activation(st[p].t[:, 4:4 + nt], st[p].t[:, 0:nt], AF.Ln, bias=EPS, scale=1.0 / D),
                 reads=[st[p]], writes=[st[p]])
            S.op("act", lambda e, p=p, nt=nt: e.activation(st[p].t[:, 8:8 + nt], st[p].t[:, 4:4 + nt], AF.Exp, scale=-0.5),
                 reads=[st[p]], writes=[st[p]])
            for i in range(nt):
                S.op("dve", lambda e, p=p, i=i: e.tensor_scalar(xh[p].t[:, i, :], xt[p].t[:, i, :], st[p].t[:, 8 + i:9 + i], None, ALU.mult),
                     reads=[xt[p], st[p]], writes=[xh[p]])

        prep(0)
        for bi, (kind, ib) in enumerate(blocks):
            p = bi % NB
            kind, nt, tok0 = blk_geom(bi)
            ntok = nt * 128
            acol = 8 if kind == "ctx" else 0
            bj = 1 if kind == "ctx" else 0
            for k2 in range(4):
                pt = pst[k2 % 2]
                for kk in range(2):
                    kc = 2 * k2 + kk
                    for i in range(nt):
                        S.op("pe", lambda e, pt=pt, kk=kk, i=i, p=p, kc=kc: e.transpose(
                            pt.t[:, kk, i * 128:(i + 1) * 128], xh[p].t[:, i, kc * 128:(kc + 1) * 128], K.identB.t[:, :]),
                            reads=[xh[p], K.identB], writes=[pt], inc=(kk == 1 and i == nt - 1))
                for kk in range(2):
                    kc = 2 * k2 + kk
                    eng = "act" if kk == 0 else "dve"
                    if eng == "act":
                        S.op("act", lambda e, pt=pt, kk=kk, p=p, kc=kc, ntok=ntok, acol=acol, bj=bj: e.activation(
                            xm[p].t[:, kc, 0:ntok], pt.t[:, kk, 0:ntok], AF.Identity,
                            scale=v.t[:, acol + kc:acol + kc + 1], bias=mf.t[:, kc, bj:bj + 1]),
                            reads=[pt, v, mf], writes=[xm[p]])
                    else:
                        S.op("dve", lambda e, pt=pt, kk=kk, p=p, kc=kc, ntok=ntok, acol=acol, bj=bj: e.tensor_scalar(
                            xm[p].t[:, kc, 0:ntok], pt.t[:, kk, 0:ntok],
                            v.t[:, acol + kc:acol + kc + 1], mf.t[:, kc, bj:bj + 1], ALU.mult, ALU.add),
                            reads=[pt, v, mf], writes=[xm[p]])

            if bi + 1 < len(blocks):
                prep(bi + 1)

            def fm_mm(ps, chunk, p=p, ntok=ntok):
                for kc in range(8):
                    S.op("pe", lambda e, kc=kc: e.matmul(ps.t[:, 0:ntok], Wb.t[:, kc, chunk * 128:(chunk + 1) * 128],
                                                        xm[p].t[:, kc, 0:ntok], start=(kc == 0), stop=(kc == 7)),
                         reads=[Wb, xm[p]], writes=[ps], inc=(kc == 7))

            for h in range(4):
                for which in ((0, 1) if kind == "own" else (1,)):
                    base = 4 * h + 2 * which
                    slot = 2 * h + which
                    if kind == "ctx":
                        ps = psf[nps % 4]; nps += 1
                        fm_mm(ps, base)
                        S.op("act", lambda e, ps=ps, p=p, slot=slot, ntok=ntok: e.copy(qk[p].t[:, slot, 0:ntok], ps.t[:, 0:ntok]),
                             reads=[ps], writes=[qk[p]])
                        continue
                    psa = psf[nps % 4]; nps += 1
                    psb = psf[nps % 4]; nps += 1
                    fm_mm(psa, base)
                    fm_mm(psb, base + 1)
                    ta = t1[slot % 2]
                    tb = t2[slot % 2]
                    S.op("dve", lambda e, psa=psa, ta=ta, p=p: e.tensor_tensor(ta.t[:, :], psa.t[:, :], cs[p].t[:, 0, :], ALU.mult),
                         reads=[psa, cs[p]], writes=[ta])
                    S.op("dve", lambda e, psb=psb, tb=tb, p=p: e.tensor_tensor(tb.t[:, :], psb.t[:, :], cs[p].t[:, 1, :], ALU.mult),
                         reads=[psb, cs[p]], writes=[tb])
                    S.op("pool", lambda e, ta=ta, tb=tb, p=p, slot=slot: e.tensor_tensor(qk[p].t[:, slot, :], ta.t[:, :], tb.t[:, :], ALU.add),
                         reads=[ta, tb], writes=[qk[p]])
            for h in range(4):
                if kind == "own":
                    S.dma("sp", K.QT[h, :, tok0:tok0 + 512], qk[p].t[:, 2 * h, :], reads=[qk[p]])
                S.dma("sp", K.KT[h, :, tok0:tok0 + ntok], qk[p].t[:, 2 * h + 1, 0:ntok], reads=[qk[p]])
            if kind == "own":
                for c4 in range(4):
                    ps = psf[nps % 4]; nps += 1
                    fm_mm(ps, 16 + c4)
                    if c4 < 2:
                        S.op("act", lambda e, ps=ps, p=p, c4=c4: e.copy(fmo[p].t[:, c4, :], ps.t[:, :]), reads=[ps], writes=[fmo[p]])
                    else:
                        S.op("act", lambda e, ps=ps, p=p, c4=c4: e.mul(fmo[p].t[:, c4, :], ps.t[:, :], 0.125), reads=[ps], writes=[fmo[p]])
                for c2 in range(2):
                    S.dma("sp", K.MQT[c2, :, tok0:tok0 + 512], fmo[p].t[:, c2, :], reads=[fmo[p]])
                    S.dma("sp", K.MKT[c2, :, tok0:tok0 + 512], fmo[p].t[:, 2 + c2, :], reads=[fmo[p]])
            for i in range(nt):
                groups = [(C_DV, 512, 0), (C_MV, 512, 512)]
                if kind == "own":
                    groups.append((C_MO, 512, 1024))
                groups.append((C_MK, 272, 1536))
                for (c0, n, o0) in groups:
                    ps = pso[npo % 2]; npo += 1
                    for kc in range(8):
                        S.op("pe", lambda e, ps=ps, kc=kc, i=i, c0=c0, n=n, p=p: e.matmul(
                            ps.t[:, 0:n], xm[p].t[:, kc, i * 128:(i + 1) * 128], Wb.t[:, kc, c0:c0 + n],
                            start=(kc == 0), stop=(kc == 7)), reads=[Wb, xm[p]], writes=[ps], inc=(kc == 7))
                    if c0 == C_MO:
                        S.op("act", lambda e, ps=ps, p=p, i=i, o0=o0: e.activation(tmo[p].t[:, i, o0:o0 + 512], ps.t[:, :], AF.Sigmoid),
                             reads=[ps], writes=[tmo[p]])
                    elif c0 == C_MK:
                        S.op("dve", lambda e, ps=ps, p=p, i=i, o0=o0: e.tensor_scalar(tmo[p].t[:, i, o0:o0 + 256], ps.t[:, 0:256], 0.125, None, ALU.mult),
                             reads=[ps], writes=[tmo[p]])
                        S.op("dve", lambda e, ps=ps, p=p, i=i: e.tensor_tensor(gto[p].t[:, i, :], ps.t[:, 256:272], sm.t[:, SM_BG:SM_BG + 16], ALU.add),
                             reads=[ps, sm], writes=[gto[p]])
                    elif c0 == C_DV:
                        S.op("dve", lambda e, ps=ps, p=p, i=i, o0=o0: e.tensor_copy(tmo[p].t[:, i, o0:o0 + 512], ps.t[:, :]),
                             reads=[ps], writes=[tmo[p]])
                    else:
                        S.op("act", lambda e, ps=ps, p=p, i=i, o0=o0: e.copy(tmo[p].t[:, i, o0:o0 + 512], ps.t[:, :]),
                             reads=[ps], writes=[tmo[p]])
            rows = slice(tok0, tok0 + ntok)
            S.dma("sp", K.V[rows, :].rearrange("(i p) c -> p i c", p=128), tmo[p].t[:, 0:nt, 0:512], reads=[tmo[p]])
            S.dma("sp", K.MV[rows, :].rearrange("(i p) c -> p i c", p=128), tmo[p].t[:, 0:nt, 512:1024], reads=[tmo[p]])
            if kind == "own":
                S.dma("sp", K.MO[rows, :].rearrange("(i p) c -> p i c", p=128), tmo[p].t[:, 0:nt, 1024:1536], reads=[tmo[p]])
            S.dma("sp", K.MK[rows, :].rearrange("(i p) c -> p i c", p=128), tmo[p].t[:, 0:nt, 1536:1792], reads=[tmo[p]])
            S.dma("sp", K.G[rows, :].rearrange("(i p) c -> p i c", p=128), gto[p].t[:, 0:nt, :], reads=[gto[p]])
        S.flush()
    S.mes = S.es


def phase2(K):
    S = K.S
    sm = K.sm
    with ExitStack() as mes:
        S.mes = mes
        RS = S.sb("RS", [128, NT, 4, 2], F32)
        WW = S.sb("WW", [128, NT, 4, 2], F32)
        CS = S.sb("CS", [128, NT, 4, 2], F32)
        EE = S.sb("EE", [128, NT, 4, 2], F32)
        with ExitStack() as sub:
            S.mes = sub
            G = S.sb("G", [128, NT, 16], F32)
            S.dma("sp", G.t[:, :, :], K.G.rearrange("(j p) g -> p j g", p=128), writes=[G])
            spl = S.sb("spl", [128, NT, 16], F32)
            S.op("act", lambda e: e.activation(spl.t[:, :, :], G.t[:, :, :], AF.Exp, scale=-1.0), reads=[G], writes=[spl])
            S.op("act", lambda e: e.activation(spl.t[:, :, :], spl.t[:, :, :], AF.Ln, bias=1.0), reads=[spl], writes=[spl])
            spc = S.sb("spc", [128, NT, 4], F32)
            a1 = S.sb("a1", [128, NT, 4], F32)
            a2 = S.sb("a2", [128, NT, 4], F32)
            psc = S.ps("psc", [128, 512], F32)
            pst_ = S.ps("pstot", [128, 512], F32)
            NN = NT * 4
            for d in range(2):
                tri = sm.t[:, SM_TRIF:SM_TRIF + 128] if d == 0 else sm.t[:, SM_TRIB:SM_TRIB + 128]
                g0 = 8 * d
                S.op("dve", lambda e, g0=g0: e.tensor_copy(spc.t[:, :, :], spl.t[:, :, g0 + 4:g0 + 8]), reads=[spl], writes=[spc])
                spc2 = spc.t[:, :, :].rearrange("p j h -> p (j h)")
                S.op("pe", lambda e, tri=tri, spc2=spc2: e.matmul(psc.t[:, 0:NN], tri, spc2, start=True, stop=True), reads=[sm, spc], writes=[psc])
                S.op("pe", lambda e, spc2=spc2: e.matmul(pst_.t[:, 0:NN], K.onesF.t[:, :], spc2, start=True, stop=True), reads=[K.onesF, spc], writes=[pst_])
                cs3 = psc.t[:, 0:NN].rearrange("p (j h) -> p j h", h=4)
                tot3 = pst_.t[:, 0:NN].rearrange("p (j h) -> p j h", h=4)
                S.op("dve", lambda e, cs3=cs3, g0=g0: e.tensor_tensor(a1.t[:, :, :], cs3, G.t[:, :, g0:g0 + 4], ALU.add), reads=[psc, G], writes=[a1])
                S.op("dve", lambda e, tot3=tot3: e.tensor_tensor(a2.t[:, :, :], a1.t[:, :, :], tot3, ALU.subtract), reads=[pst_, a1], writes=[a2])
                S.op("act", lambda e, d=d: e.activation(RS.t[:, :, :, d], a1.t[:, :, :], AF.Exp), reads=[a1], writes=[RS])
                S.op("act", lambda e, d=d: e.activation(WW.t[:, :, :, d], a2.t[:, :, :], AF.Exp), reads=[a2], writes=[WW])
                S.op("act", lambda e, d=d, cs3=cs3: e.activation(CS.t[:, :, :, d], cs3, AF.Exp, scale=-1.0), reads=[psc], writes=[CS])
                S.op("act", lambda e, d=d, tot3=tot3: e.activation(EE.t[:, :, :, d], tot3, AF.Exp, scale=-1.0), reads=[pst_], writes=[EE])
            S.flush()
        S.mes = mes
        mk_tok = S.sb("mk_tok", [128, NT, 64], BF16)
        v_aug = S.sb("v_aug", [128, NT, 129], BF16)
        mqT = S.sb("mqT", [64, TOWN], BF16)
        mkT = S.sb("mkT", [64, TOWN], BF16)
        mo_h = S.sb("mo_h", [128, 32, 128], BF16)
        kw = [S.sb("kwF", [128, NT, 64], BF16), S.sb("kwR", [128, NT, 64], BF16)]
        Cs = [S.sb("CsF", [64, 34, 129], F32), S.sb("CsR", [64, 34, 129], F32)]
        Cb = [S.sb("CbF", [64, 32, 129], BF16), S.sb("CbR", [64, 32, 129], BF16)]
        Pm = [[S.sb(f"P{d}{i}", [128, 128], BF16) for i in range(2)] for d in range(2)]
        sml = [S.sb(f"sml{i}", [128, 16], F32) for i in range(2)]
        hF = [S.sb(f"hF{i}", [128, 128], F32) for i in range(2)]
        hs = [S.sb(f"hs{i}", [128, 128], F32) for i in range(2)]
        junk = S.sb("junk2", [128, 128], BF16)
        mlb = [S.sb(f"mlb{i}", [128, 128], BF16) for i in range(2)]
        mlT = [S.sb(f"mlT{i}", [128, 4, 128], BF16) for i in range(2)]
        psU = [S.ps(f"psU{i}", [128, 512], F32) for i in range(2)]
        psS = [S.ps(f"psS{i}", [128, 128], F32) for i in range(2)]
        psH = [S.ps(f"psH{i}", [128, 2, 129], F32) for i in range(2)]
        psT = [S.ps(f"psT{i}", [128, 128], BF16) for i in range(2)]
        S.op("pool", lambda e: e.memset(v_aug.t[:, :, 128:129], 1.0), writes=[v_aug])
        seqs = [[64, 65] + list(range(0, 31)), [65, 64] + list(range(63, 31, -1)) + list(range(31, 0, -1))]
        nU = 0
        for h in range(4):
            S.dma("sp", mk_tok.t[:, :, :], K.MK[:, h * 64:(h + 1) * 64].rearrange("(j p) d -> p j d", p=128), writes=[mk_tok])
            S.dma("sp", v_aug.t[:, :, 0:128], K.MV[:, h * 128:(h + 1) * 128].rearrange("(j p) d -> p j d", p=128), writes=[v_aug])
            r0 = (h % 2) * 64
            S.dma("sp", mqT.t[:, :], K.MQT[h // 2, r0:r0 + 64, :], writes=[mqT])
            S.dma("sp", mkT.t[:, :], K.MKT[h // 2, r0:r0 + 64, :], writes=[mkT])
            S.dma("sp", mo_h.t[:, :, :], K.MO[:, h * 128:(h + 1) * 128].rearrange("(j p) d -> p j d", p=128), writes=[mo_h])
            for d in range(2):
                S.op("dve", lambda e, d=d, h=h: e.tensor_tensor(kw[d].t[:, :, :], mk_tok.t[:, :, :],
                                                             WW.t[:, :, h, d:d + 1].to_broadcast([128, NT, 64]), ALU.mult),
                     reads=[mk_tok, WW], writes=[kw[d]])
            for d in range(2):
                seq = seqs[d]
                npre = len(seq) - 31
                prev = None
                for si, j in enumerate(seq):
                    u = si % 3
                    if u == 0:
                        pu = psU[nU % 2]; nU += 1
                        grp = seq[si:si + 3]
                        for gi, jj in enumerate(grp):
                            S.op("pe", lambda e, pu=pu, gi=gi, jj=jj, d=d: e.matmul(
                                pu.t[0:64, gi * 129:(gi + 1) * 129], kw[d].t[:, jj, :], v_aug.t[:, jj, :], start=True, stop=True),
                                reads=[kw[d], v_aug], writes=[pu], inc=(gi == len(grp) - 1))
                    if si < npre - 1:
                        dst = si % 2
                    else:
                        dst = 2 + (si - (npre - 1))
                    uap = pu.t[0:64, u * 129:(u + 1) * 129]
                    if prev is None:
                        S.op("dve", lambda e, d=d, dst=dst, uap=uap: e.tensor_copy(Cs[d].t[:, dst, :], uap), reads=[pu], writes=[Cs[d]])
                    else:
                        S.op("dve", lambda e, d=d, dst=dst, uap=uap, prev=prev, j=j, h=h: e.scalar_tensor_tensor(
                            Cs[d].t[:, dst, :], Cs[d].t[:, prev, :], EE.t[0:64, j, h, d:d + 1], uap, ALU.mult, ALU.add),
                            reads=[pu, Cs[d], EE], writes=[Cs[d]])
                    prev = dst
                S.op("pool", lambda e, d=d: e.tensor_copy(Cb[d].t[:, :, :], Cs[d].t[:, 2:34, :]), reads=[Cs[d]], writes=[Cb[d]])
            for j in range(32):
                q = j % 2
                tsl = slice(j * 128, (j + 1) * 128)
                S.op("pe", lambda e, q=q, tsl=tsl: e.matmul(psS[q].t[:, :], mkT.t[:, tsl], mqT.t[:, tsl], start=True, stop=True),
                     reads=[mkT, mqT], writes=[psS[q]])
                for d in range(2):
                    tri = sm.t[:, SM_TRIF:SM_TRIF + 128] if d == 0 else sm.t[:, SM_TRIB:SM_TRIB + 128]
                    S.op("dve", lambda e, d=d, q=q, j=j, h=h, tri=tri: e.scalar_tensor_tensor(
                        Pm[d][q].t[:, :], psS[q].t[:, :], RS.t[:, j, h, d:d + 1], tri, ALU.mult, ALU.mult),
                        reads=[psS[q], RS, sm], writes=[Pm[d][q]])
                for d in range(2):
                    slot = j if d == 0 else 31 - j
                    S.op("pe", lambda e, d=d, q=q, j=j: e.matmul(psH[q].t[:, d, :], Pm[d][q].t[:, :], v_aug.t[:, j, :], start=True, stop=False),
                         reads=[Pm[d][q], v_aug], writes=[psH[q]], inc=False)
                    S.op("pe", lambda e, d=d, q=q, tsl=tsl, slot=slot: e.matmul(psH[q].t[:, d, :], mqT.t[:, tsl], Cb[d].t[:, slot, :], start=False, stop=True),
                         reads=[mqT, Cb[d]], writes=[psH[q]], inc=(d == 1))
                sq = sml[q]
                S.op("dve", lambda e, q=q, sq=sq, j=j, h=h: e.tensor_tensor(sq.t[:, 0:2], psH[q].t[:, :, 128], CS.t[:, j, h, :], ALU.mult),
                     reads=[psH[q], CS], writes=[sq])
                S.op("dve", lambda e, sq=sq: e.scalar_tensor_tensor(sq.t[:, 2:4], sq.t[:, 0:2], -1.0, sq.t[:, 0:2], ALU.mult, ALU.max), reads=[sq], writes=[sq])
                S.op("dve", lambda e, sq=sq: e.tensor_scalar(sq.t[:, 2:4], sq.t[:, 2:4], 1.0, None, ALU.max), reads=[sq], writes=[sq])
                S.op("dve", lambda e, sq=sq: e.reciprocal(sq.t[:, 4:6], sq.t[:, 2:4]), reads=[sq], writes=[sq])
                S.op("dve", lambda e, sq=sq, j=j, h=h: e.tensor_tensor(sq.t[:, 6:8], sq.t[:, 4:6], CS.t[:, j, h, :], ALU.mult), reads=[sq, CS], writes=[sq])
                S.op("dve", lambda e, q=q, sq=sq: e.tensor_scalar(hF[q].t[:, :], psH[q].t[:, 0, 0:128], sq.t[:, 6:7], None, ALU.mult),
                     reads=[psH[q], sq], writes=[hF[q]])
                S.op("dve", lambda e, q=q, sq=sq: e.scalar_tensor_tensor(hs[q].t[:, :], psH[q].t[:, 1, 0:128], sq.t[:, 7:8], hF[q].t[:, :], ALU.mult, ALU.add),
                     reads=[psH[q], sq, hF[q]], writes=[hs[q]])
                S.op("act", lambda e, q=q, sq=sq: e.activation(junk.t[:, :], hs[q].t[:, :], AF.Square, accum_out=sq.t[:, 8:9]),
                     reads=[hs[q]], writes=[junk, sq])
                S.op("act", lambda e, sq=sq: e.activation(sq.t[:, 9:10], sq.t[:, 8:9], AF.Ln, bias=EPS, scale=1.0 / 128), reads=[sq], writes=[sq])
                S.op("act", lambda e, sq=sq: e.activation(sq.t[:, 10:11], sq.t[:, 9:10], AF.Exp, scale=-0.5), reads=[sq], writes=[sq])
                S.op("dve", lambda e, q=q, sq=sq, j=j: e.scalar_tensor_tensor(mlb[q].t[:, :], hs[q].t[:, :], sq.t[:, 10:11], mo_h.t[:, j, :], ALU.mult, ALU.mult),
                     reads=[hs[q], sq, mo_h], writes=[mlb[q]])
                S.op("pe", lambda e, q=q: e.transpose(psT[q].t[:, :], mlb[q].t[:, :], K.identB.t[:, :]), reads=[mlb[q], K.identB], writes=[psT[q]])
                mt = mlT[(j // 4) % 2]
                S.op("act", lambda e, q=q, mt=mt, j=j: e.copy(mt.t[:, j % 4, :], psT[q].t[:, :]), reads=[psT[q]], writes=[mt])
                if j % 4 == 3:
                    j0 = j - 3
                    S.dma("sp", K.MIXT[4 + h, :, j0 * 128:(j0 + 4) * 128], mt.t[:, :, :].rearrange("p a b -> p (a b)"), reads=[mt])
        S.flush()
    S.mes = S.es


def phase3(K):
    S = K.S
    v = K.vec
    NQB = TOWN // 512
    with ExitStack() as mes:
        S.mes = mes
        KTh = [S.sb(f"KTh{i}", [128, NKEY], BF16) for i in range(2)]
        Vh = [S.sb(f"Vh{i}", [128, NT, 128], BF16) for i in range(2)]
        QTh = [S.sb(f"QTh{i}", [128, TOWN], BF16) for i in range(2)]
        NE = 4
        E = [S.sb(f"E{i}", [128, 1024], BF16) for i in range(NE)]
        sel1 = S.sb("sel1", [128, 128], F32)
        sel2 = S.sb("sel2", [128, 128], F32)
        zs = S.sb("zs", [128, 512], F32)
        idf = K.sm.t[:, SM_ID:SM_ID + 128]
        S.op("dve", lambda e: e.tensor_tensor(sel1.t[:, :], idf[:, 0:1].to_broadcast([128, 128]), idf[:, 64:65].to_broadcast([128, 128]), ALU.add),
             reads=[K.sm], writes=[sel1])
        S.op("dve", lambda e: e.tensor_tensor(sel2.t[:, :], idf[:, 32:33].to_broadcast([128, 128]), idf[:, 96:97].to_broadcast([128, 128]), ALU.add),
             reads=[K.sm], writes=[sel2])
        S.op("dve", lambda e: e.tensor_tensor(sel1.t[:, :], sel1.t[:, :], sel2.t[:, :], ALU.add), reads=[sel1, sel2], writes=[sel1])
        acc2 = [S.sb(f"acc2_{i}", [128, 512], F32) for i in range(2)]
        rr = S.sb("rr", [128, 1024], F32)
        o1 = S.sb("o1", [128, 512], F32)
        o2 = S.sb("o2", [128, 512], F32)
        oo = S.sb("oo", [128, 512], F32)
        sq = S.sb("sq", [128, 512], F32)
        lnv = S.sb("lnv", [128, 512], F32)
        rstd = S.sb("rstd", [128, 512], F32)
        dao = [S.sb(f"dao{i}", [128, 512], BF16) for i in range(2)]
        psS = [S.ps(f"psS{i}", [128, 1024], F32) for i in range(2)]
        psO = S.ps("psO", [128, 1024], F32)
        psZ = S.ps("psZ", [128, 512], F32)
        psB = S.ps("psB", [128, 512], F32)

        def load_head(h):
            b = h % 2
            S.dma("sp", KTh[b].t[:, :], K.KT[h, :, :], writes=[KTh[b]])
            S.dma("sp", Vh[b].t[:, :, :], K.V[:, h * 128:(h + 1) * 128].rearrange("(j p) d -> p j d", p=128), writes=[Vh[b]])
            S.dma("sp", QTh[b].t[:, :], K.QT[h, :, :], writes=[QTh[b]])

        pendB = []

        def attn_block(h, b, qb, it, nq):
            qs = slice(qb * 512, (qb + 1) * 512)
            ac2 = acc2[nq % 2]

            def qk(kb, itn):
                ps = psS[itn % 2]
                ks = slice(kb * 128, (kb + 1) * 128)
                S.op("pe", lambda e: e.matmul(ps.t[:, 0:512], KTh[b].t[0:64, ks], QTh[b].t[0:64, qs], start=True, stop=True),
                     reads=[KTh[b], QTh[b]], writes=[ps], inc=False)
                S.op("pe", lambda e: e.matmul(ps.t[:, 512:1024], KTh[b].t[64:128, ks], QTh[b].t[64:128, qs], start=True, stop=True),
                     reads=[KTh[b], QTh[b]], writes=[ps])

            qk(0, it)
            for kb in range(NT):
                itn = it + kb
                if kb + 1 < NT:
                    qk(kb + 1, itn + 1)
                if kb == 6 and pendB:
                    pendB.pop(0)()
                ps = psS[itn % 2]
                Et = E[itn % NE]
                S.op("act", lambda e, ps=ps, Et=Et: e.activation(Et.t[:, :], ps.t[:, :], AF.Exp, scale=0.125), reads=[ps], writes=[Et])
                S.op("pe", lambda e, Et=Et, kb=kb: e.matmul(psO.t[:, 0:512], Vh[b].t[:, kb, :], Et.t[:, 0:512], start=(kb == 0), stop=(kb == NT - 1)),
                     reads=[Vh[b], Et], writes=[psO], inc=False)
                S.op("pe", lambda e, Et=Et, kb=kb: e.matmul(psO.t[:, 512:1024], Vh[b].t[:, kb, :], Et.t[:, 512:1024], start=(kb == 0), stop=(kb == NT - 1)),
                     reads=[Vh[b], Et], writes=[psO])
                S.op("pe", lambda e, Et=Et, kb=kb: e.matmul(
                    psZ.t[:, :], K.onesB.t[:, :], Et.t[:, 0:512], start=(kb == 0), stop=(kb == NT - 1)),
                    reads=[K.onesB, Et], writes=[psZ])
                if kb == 0:
                    S.op("dve", lambda e, Et=Et: e.tensor_copy(ac2.t[:, :], Et.t[:, 512:1024]), reads=[Et], writes=[ac2])
                else:
                    S.op("dve", lambda e, Et=Et: e.tensor_tensor(ac2.t[:, :], ac2.t[:, :], Et.t[:, 512:1024], ALU.add), reads=[Et, ac2], writes=[ac2])
            it += NT
            S.op("pe", lambda e: e.matmul(psB.t[:, :], K.onesF.t[:, :], ac2.t[:, :], start=True, stop=True), reads=[K.onesF, ac2], writes=[psB])
            S.op("dve", lambda e: e.reciprocal(rr.t[:, 0:512], psZ.t[:, :]), reads=[psZ], writes=[rr])
            S.op("dve", lambda e: e.reciprocal(rr.t[:, 512:1024], psB.t[:, :]), reads=[psB], writes=[rr])
            S.op("dve", lambda e: e.tensor_tensor(o1.t[:, :], psO.t[:, 0:512], rr.t[:, 0:512], ALU.mult), reads=[psO, rr], writes=[o1])
            S.op("dve", lambda e: e.tensor_tensor(o2.t[:, :], psO.t[:, 512:1024], rr.t[:, 512:1024], ALU.mult), reads=[psO, rr], writes=[o2])
            dd = dao[nq % 2]

            def part_b():
                S.op("dve", lambda e: e.scalar_tensor_tensor(oo.t[:, :], o2.t[:, :], v.t[:, 25:26], o1.t[:, :], ALU.mult, ALU.add), reads=[o1, o2, v], writes=[oo])
                S.op("act", lambda e: e.activation(sq.t[:, :], oo.t[:, :], AF.Square), reads=[oo], writes=[sq])
                S.op("pe", lambda e: e.matmul(psB.t[:, :], K.onesF.t[:, :], sq.t[:, :], start=True, stop=True), reads=[K.onesF, sq], writes=[psB])
                S.op("act", lambda e: e.activation(lnv.t[:, :], psB.t[:, :], AF.Ln, bias=EPS, scale=1.0 / 128), reads=[psB], writes=[lnv])
                S.op("act", lambda e: e.activation(rstd.t[:, :], lnv.t[:, :], AF.Exp, scale=-0.5), reads=[lnv], writes=[rstd])
                S.op("dve", lambda e: e.scalar_tensor_tensor(dd.t[:, :], oo.t[:, :], v.t[:, 26:27], rstd.t[:, :], ALU.mult, ALU.mult), reads=[oo, rstd, v], writes=[dd])
                S.dma("sp", K.MIXT[h, :, qs], dd.t[:, :], reads=[dd])

            pendB.append(part_b)
            nq += 1
            return it, nq

        load_head(0)
        it = 0
        nq = 0
        for h in range(4):
            b = h % 2
            if h + 1 < 4:
                load_head(h + 1)
            for qb in range(NQB):
                it, nq = attn_block(h, b, qb, it, nq)
        while pendB:
            pendB.pop(0)()
        S.flush()
    S.mes = S.es


def phase4(K):
    S = K.S
    sm, v, mf, bc = K.sm, K.vec, K.modfm, K.bc
    with ExitStack() as mes:
        S.mes = mes
        Wo = S.sb("Wo", [128, 8, D], BF16)
        W1 = S.sb("W1", [128, 8, DFF], BF16)
        W2 = S.sb("W2", [128, 32, D], BF16)
        S.dma("pool", Wo.t[:, 0:4, :], K.w_out[0:512, :].rearrange("(k p) f -> p k f", p=128), writes=[Wo])
        with ExitStack() as sub:
            S.mes = sub
            wof = S.sb("wof", [128, 4, D], F32)
            S.dma("sp", wof.t[:, :, :], K.w_out[512:1024, :].rearrange("(k p) f -> p k f", p=128), writes=[wof])
            for hh in range(4):
                S.op("dve", lambda e, hh=hh: e.tensor_scalar(Wo.t[:, 4 + hh, :], wof.t[:, hh, :], sm.t[:, SM_MLG + hh:SM_MLG + hh + 1], None, ALU.mult),
                     reads=[wof, sm], writes=[Wo])
            S.flush()
        S.mes = mes
        for kc in range(8):
            S.dma("pool", W1.t[:, kc, :], K.w_fc1[kc * 128:(kc + 1) * 128, :], writes=[W1])
        for g in range(4):
            S.dma("pool", W2.t[:, 8 * g:8 * (g + 1), :], K.w_fc2[1024 * g:1024 * (g + 1), :].rearrange("(k p) f -> p k f", p=128), writes=[W2])
        xt2 = [S.sb(f"x2_{i}", [128, 2, D], F32) for i in range(2)]
        mix = [S.sb(f"mix{i}", [128, 8, 256], BF16) for i in range(2)]
        xn = S.sb("xn", [128, 2, D], BF16)
        xm2 = S.sb("xm2", [128, 8, 256], BF16)
        tmp = [S.sb(f"tmp{i}", [128, 512], F32) for i in range(2)]
        r1 = [S.sb(f"r1{i}", [128, 256], F32) for i in range(3)]
        h1 = [S.sb(f"h1{i}", [128, 256], BF16) for i in range(4)]
        st4 = [S.sb(f"st4{i}", [128, 16], F32) for i in range(2)]
        junk = S.sb("junk4", [128, D], BF16)
        psF = [[S.ps(f"psF{i}{hf}", [128, 512], F32) for hf in range(2)] for i in range(2)]
        pst = S.ps("pst4", [128, 8, 128], BF16)
        ps1 = [S.ps(f"ps1{i}", [128, 256], F32) for i in range(3)]
        nblk = TOWN // 256
        ntmp = 0
        nf = 0

        def load_blk(bi):
            p = bi % 2
            t0 = bi * 256
            S.dma("sp", xt2[p].t[:, :, :], K.x[t0:t0 + 256, :].rearrange("(i p) d -> p i d", p=128), writes=[xt2[p]])
            S.dma("sp", mix[p].t[:, :, :], K.MIXT[:, :, t0:t0 + 256].rearrange("c p t -> p c t"), writes=[mix[p]])

        cnt = {'ntmp': 0, 'nf': 0}

        def do_block(bi, p, t0):
            ntmp = cnt['ntmp']
            nf = cnt['nf']
            X = xt2[p]
            sst = st4[p]
            for i in range(2):
                for hf in range(2):
                    ps = psF[i][hf]
                    hs_ = slice(hf * 512, (hf + 1) * 512)
                    for c in range(8):
                        S.op("pe", lambda e, ps=ps, c=c, i=i, hs_=hs_: e.matmul(ps.t[:, :], mix[p].t[:, c, i * 128:(i + 1) * 128], Wo.t[:, c, hs_],
                                                                           start=(c == 0), stop=(c == 7)),
                             reads=[mix[p], Wo], writes=[ps], inc=(c == 7))
                    tm = tmp[ntmp % 2]; ntmp += 1
                    S.op("dve", lambda e, ps=ps, tm=tm, hs_=hs_: e.tensor_tensor(tm.t[:, :], ps.t[:, :], bc.t[:, 0, hs_], ALU.mult), reads=[ps, bc], writes=[tm])
                    S.op("pool", lambda e, tm=tm, i=i, hs_=hs_: e.tensor_tensor(X.t[:, i, hs_], tm.t[:, :], X.t[:, i, hs_], ALU.add), reads=[tm, X], writes=[X])
            for i in range(2):
                S.op("act", lambda e, i=i: e.activation(junk.t[:, :], X.t[:, i, :], AF.Square, accum_out=sst.t[:, i:i + 1]), reads=[X], writes=[junk, sst])
            S.op("act", lambda e: e.activation(sst.t[:, 2:4], sst.t[:, 0:2], AF.Ln, bias=EPS, scale=1.0 / D), reads=[sst], writes=[sst])
            S.op("act", lambda e: e.activation(sst.t[:, 4:6], sst.t[:, 2:4], AF.Exp, scale=-0.5), reads=[sst], writes=[sst])
            for i in range(2):
                S.op("dve", lambda e, i=i: e.tensor_scalar(xn.t[:, i, :], X.t[:, i, :], sst.t[:, 4 + i:5 + i], None, ALU.mult), reads=[X, sst], writes=[xn])
            for i in range(2):
                for kc in range(8):
                    S.op("pe", lambda e, i=i, kc=kc: e.transpose(pst.t[:, kc, :], xn.t[:, i, kc * 128:(kc + 1) * 128], K.identB.t[:, :]),
                         reads=[xn, K.identB], writes=[pst], inc=(kc == 7))
                for kc in range(8):
                    if kc % 2 == 0:
                        S.op("act", lambda e, i=i, kc=kc: e.activation(xm2.t[:, kc, i * 128:(i + 1) * 128], pst.t[:, kc, :], AF.Identity,
                                                                     scale=v.t[:, 16 + kc:17 + kc], bias=mf.t[:, 24 + kc, 0:1]),
                             reads=[pst, v, mf], writes=[xm2])
                    else:
                        S.op("dve", lambda e, i=i, kc=kc: e.tensor_scalar(xm2.t[:, kc, i * 128:(i + 1) * 128], pst.t[:, kc, :],
                                                                        v.t[:, 16 + kc:17 + kc], mf.t[:, 24 + kc, 0:1], ALU.mult, ALU.add),
                             reads=[pst, v, mf], writes=[xm2])
            def ffn1(f, nfi):
                p1 = ps1[nfi % 3]
                rt = r1[nfi % 3]
                ht = h1[nfi % 4]
                for kc in range(8):
                    S.op("pe", lambda e, kc=kc: e.matmul(p1.t[:, :], W1.t[:, kc, f * 128:(f + 1) * 128], xm2.t[:, kc, :], start=(kc == 0), stop=(kc == 7)),
                         reads=[W1, xm2], writes=[p1], inc=(kc == 7))
                S.op("act", lambda e: e.activation(rt.t[:, :], p1.t[:, :], AF.Relu), reads=[p1], writes=[rt])
                S.op("dve", lambda e: e.tensor_tensor(ht.t[:, :], rt.t[:, :], rt.t[:, :], ALU.mult), reads=[rt], writes=[ht])
                return ht

            def ffn2(f, ht):
                for i in range(2):
                    for hf in range(2):
                        S.op("pe", lambda e, i=i, hf=hf: e.matmul(psF[i][hf].t[:, :], ht.t[:, i * 128:(i + 1) * 128], W2.t[:, f, hf * 512:(hf + 1) * 512],
                                                                  start=(f == 0), stop=(f == 31)),
                             reads=[ht, W2], writes=[psF[i][hf]], inc=(i == 1 and hf == 1))

            hts = {}
            hts[0] = ffn1(0, nf)
            hts[1] = ffn1(1, nf + 1)
            for f in range(32):
                if f + 2 < 32:
                    hts[f + 2] = ffn1(f + 2, nf + f + 2)
                ffn2(f, hts.pop(f))
            nf += 32
            for i in range(2):
                for hf in range(2):
                    hs_ = slice(hf * 512, (hf + 1) * 512)
                    tm = tmp[ntmp % 2]; ntmp += 1
                    S.op("dve", lambda e, i=i, hf=hf, tm=tm, hs_=hs_: e.tensor_tensor(tm.t[:, :], psF[i][hf].t[:, :], bc.t[:, 1, hs_], ALU.mult),
                         reads=[psF[i][hf], bc], writes=[tm])
                    S.op("pool", lambda e, tm=tm, i=i, hs_=hs_: e.tensor_tensor(X.t[:, i, hs_], tm.t[:, :], X.t[:, i, hs_], ALU.add), reads=[tm, X], writes=[X])
            for i in range(2):
                S.op("act", lambda e, i=i: e.activation(junk.t[:, :], X.t[:, i, :], AF.Square, accum_out=sst.t[:, 8 + i:9 + i]), reads=[X], writes=[junk, sst])
            S.op("act", lambda e: e.activation(sst.t[:, 10:12], sst.t[:, 8:10], AF.Ln, bias=EPS, scale=1.0 / D), reads=[sst], writes=[sst])
            S.op("act", lambda e: e.activation(sst.t[:, 12:14], sst.t[:, 10:12], AF.Exp, scale=-0.5), reads=[sst], writes=[sst])
            for i in range(2):
                S.op("dve", lambda e, i=i: e.scalar_tensor_tensor(X.t[:, i, :], X.t[:, i, :], sst.t[:, 12 + i:13 + i], bc.t[:, 2, :], ALU.mult, ALU.mult),
                     reads=[X, sst, bc], writes=[X])
            S.dma("sp", K.out[t0:t0 + 256, :].rearrange("(i p) d -> p i d", p=128), X.t[:, :, :], reads=[X], out=True)
            cnt['ntmp'] = ntmp
            cnt['nf'] = nf

        load_blk(0)
        for bi in range(nblk):
            if bi + 1 < nblk:
                load_blk(bi + 1)
            do_block(bi, bi % 2, bi * 256)
        S.flush()
    S.mes = S.es


def _rope_tables(half):
    g = np.arange(T, dtype=np.int64)
    if half == 1:
        g = g[::-1]
    row = (g // 64).astype(np.float32)
    col = (g % 64).astype(np.float32)
    inv = (np.float32(10000.0) ** (-np.arange(0, 32, 2, dtype=np.float32) / np.float32(32))).astype(np.float32)
    ar = row[:, None] * inv
    ac = col[:, None] * inv
    ang = np.concatenate([ar, ar, ac, ac], axis=-1).astype(np.float32)
    cos = np.cos(ang).astype(np.float32)
    sin = np.sin(ang).astype(np.float32)
    sign = np.where((np.arange(64) % 32) < 16, -1.0, 1.0).astype(np.float32)
    sin = sin * sign[None, :]
    cosT = np.ascontiguousarray(np.concatenate([cos.T, cos.T], axis=0))
    sinT = np.ascontiguousarray(np.concatenate([sin.T, sin.T], axis=0))
    return cosT, sinT


def _w_cols(half):
    DQ0, DK0, DV0, MQ0, MK0, MV0, MO0, MG0 = 0, 512, 1024, 1536, 1792, 2048, 2560, 3072
    partner = [d + 16 if (d % 32) < 16 else d - 16 for d in range(64)]
    cols = []
    for h in range(4):
        for base in (DQ0, DK0):
            cols += [base + h * 128 + m * 64 + d for m in range(2) for d in range(64)]
            cols += [base + h * 128 + m * 64 + partner[d] for m in range(2) for d in range(64)]
    cols += list(range(MQ0, MQ0 + 256)) + list(range(MK0, MK0 + 256))
    cols += list(range(DV0, DV0 + 512)) + list(range(MV0, MV0 + 512)) + list(range(MO0, MO0 + 512))
    cols += list(range(MK0, MK0 + 256))
    gorder = list(range(16)) if half == 0 else (list(range(8, 16)) + list(range(0, 8)))
    cols += [MG0 + g for g in gorder]
    assert len(cols) == WCOLS
    return np.asarray(cols), gorder


def host_layout(inp):
    f32 = np.float32
    x = np.asarray(inp["x"], f32)
    ctx = np.asarray(inp["ctx"], f32)
    c = np.asarray(inp["c"], f32)
    c_ctx = np.asarray(inp["c_ctx"], f32)
    w_in = np.asarray(inp["w_in"], f32)[0]
    w_ada = np.ascontiguousarray(np.asarray(inp["w_ada"], f32)[0])
    b_ada = np.asarray(inp["b_ada"], f32)[0]
    w_out = np.ascontiguousarray(np.asarray(inp["w_out"], f32)[0])
    w_fc1 = np.ascontiguousarray(np.asarray(inp["w_fc1"], f32)[0])
    w_fc2 = np.ascontiguousarray(np.asarray(inp["w_fc2"], f32)[0])
    tri = np.triu(np.ones((128, 128), f32))
    per_half = []
    for half in range(2):
        cols, gorder = _w_cols(half)
        w_loc = np.ascontiguousarray(w_in[:, cols])
        cosT, sinT = _rope_tables(half)
        per_half.append((w_loc, cosT, sinT, gorder))
    bc_in = np.empty((128, 3, D), f32)
    bc_in[:, 0, :] = b_ada[2 * D:3 * D][None, :]
    bc_in[:, 1, :] = b_ada[5 * D:6 * D][None, :]
    bc_in[:, 2, :] = np.asarray(inp["final_g"], f32)[None, :]
    maps = []
    for core in range(8):
        b, half = core // 2, core % 2
        w_loc, cosT, sinT, gorder = per_half[half]
        smalls = np.zeros((128, NS), f32)
        ct = np.stack([c[b].reshape(8, 128).T, c_ctx.reshape(8, 128).T], axis=-1)
        smalls[:, SM_CT:SM_CT + 16] = ct.reshape(128, 16)
        smalls[:, SM_BADA:SM_BADA + 48] = b_ada.reshape(48, 128).T
        smalls[:, SM_NG:SM_NG + 8] = np.asarray(inp["norm1_g"], f32)[0].reshape(8, 128).T
        smalls[:, SM_NG + 8:SM_NG + 16] = np.asarray(inp["norm2_g"], f32)[0].reshape(8, 128).T
        for i, nm in enumerate(("lam_q1", "lam_k1", "lam_q2", "lam_k2")):
            smalls[:, SM_LAM + 64 * i:SM_LAM + 64 * (i + 1)] = np.asarray(inp[nm], f32)[0][None, :]
        smalls[:, SM_SUB] = np.asarray(inp["subln_g"], f32)[0]
        smalls[:, SM_MLG:SM_MLG + 4] = np.asarray(inp["mlstm_norm_g"], f32)[0].reshape(4, 128).T
        smalls[:, SM_BG:SM_BG + 16] = np.asarray(inp["b_gate"], f32)[0][gorder][None, :]
        smalls[:, SM_TRIF:SM_TRIF + 128] = tri
        smalls[:, SM_TRIB:SM_TRIB + 128] = tri.T
        smalls[:, SM_ID:SM_ID + 128] = np.eye(128, dtype=f32)
        xl = x[b] if half == 0 else x[b][::-1]
        cl = ctx[b] if half == 0 else ctx[b][::-1]
        maps.append({
            "x_loc": np.ascontiguousarray(xl), "ctx_loc": np.ascontiguousarray(cl),
            "w_loc": w_loc, "w_ada": w_ada, "w_out": w_out, "w_fc1": w_fc1, "w_fc2": w_fc2,
            "cosT": cosT, "sinT": sinT, "smalls": smalls, "bc_in": bc_in,
        })
    return maps


_NC_CACHE = {}


def kernel(**inputs):
    maps = host_layout(inputs)
    if "nc" not in _NC_CACHE:
        _NC_CACHE["nc"] = build_program()
    nc = _NC_CACHE["nc"]
    res = run_bass_kernel_spmd(nc, maps, core_ids=list(range(8)))
    out = np.empty((4, T, D), np.float32)
    for core in range(8):
        b, half = core // 2, core % 2
        o = np.asarray(res.results[core]["out"])
        if half == 0:
            out[b, 0:TOWN] = o
        else:
            out[b, TOWN:T] = o[::-1]
    return out
```
